# Optimizing a Trainium2 kernel written in Bass

```python
import math
import jax
import jax.numpy as jnp
from jax import lax
import numpy as np

D_MODEL = 1024
BATCH = 8
SEQ = 2048
DEPTH = 2

N_EVEN = (DEPTH + 1) // 2
N_ODD = DEPTH // 2
EPS = 1e-6
ROPE_THETA = 10000.0
POS_OFFSET_MAX = 1024

GLA_HEADS = 4
GLA_DK = D_MODEL // 16
GLA_DV = D_MODEL // 8
GLA_RANK = 16
GLA_CHUNK = 64
GLA_GATE_NORMALIZER = 16.0

DIFF_HEADS = 4
DIFF_D = D_MODEL // 16
DIFF_DV = 2 * DIFF_D
ATTN_BLOCK = 128

SGU_GROUPS = 4
SGU_CH = D_MODEL // 8
SGU_CHUNK = 128

RET_HEADS = 4
RET_DK = D_MODEL // 16
RET_DV = D_MODEL // 8
RET_CHUNK = 128

N_EXPERTS = 32
TOP_K = 4
D_FF = D_MODEL
SWIGLU_ALPHA = 1.702
SWIGLU_LIMIT = 7.0
MOE_BLOCK = 256

GLA_QK_W = GLA_HEADS * GLA_DK
GLA_V_W = GLA_HEADS * GLA_DV
DIFF_QK_W = DIFF_HEADS * 2 * DIFF_D
DIFF_V_W = DIFF_HEADS * DIFF_DV
SGU_W = SGU_GROUPS * SGU_CH
RET_QK_W = RET_HEADS * RET_DK
RET_V_W = RET_HEADS * RET_DV
EVEN_WIDTHS = (GLA_QK_W, GLA_QK_W, GLA_V_W, GLA_RANK, GLA_V_W, DIFF_QK_W, DIFF_QK_W, DIFF_V_W)
ODD_WIDTHS = (SGU_W, SGU_W, RET_QK_W, RET_QK_W, RET_V_W, RET_V_W)
EVEN_IN = sum(EVEN_WIDTHS)
ODD_IN = sum(ODD_WIDTHS)
EVEN_OUT = GLA_V_W + DIFF_V_W
ODD_OUT = SGU_W + RET_V_W

kernel_name = 'hybrid_gla_diff_sgu_retention_moe'


def _split(z, widths):
    out, start = [], 0
    for w in widths:
        out.append(z[..., start:start + w])
        start += w
    return out


def _rms(x):
    xf = x.astype(jnp.float32)
    return (xf * lax.rsqrt(jnp.mean(jnp.square(xf), axis=-1, keepdims=True) + EPS)).astype(x.dtype)


def _layer_norm(x, g, b):
    xf = x.astype(jnp.float32)
    mu = jnp.mean(xf, axis=-1, keepdims=True)
    var = jnp.mean(jnp.square(xf - mu), axis=-1, keepdims=True)
    return ((xf - mu) * lax.rsqrt(var + EPS)).astype(x.dtype) * g + b


def _heads(t, n):
    B, L, _ = t.shape
    return t.reshape(B, L, n, -1).transpose(0, 2, 1, 3)


def _merge(t):
    B, H, L, e = t.shape
    return t.transpose(0, 2, 1, 3).reshape(B, L, H * e)


def _rope(x, pos):
    half = x.shape[-1] // 2
    inv_freq = ROPE_THETA ** (-jnp.arange(half, dtype=jnp.float32) / half)
    ang = pos.astype(jnp.float32)[..., None] * inv_freq
    ang = ang.reshape((ang.shape[0],) + (1,) * (x.ndim - 3) + ang.shape[1:])
    cos, sin = jnp.cos(ang), jnp.sin(ang)
    xf = x.astype(jnp.float32)
    x1, x2 = xf[..., :half], xf[..., half:]
    return jnp.concatenate([x1 * cos - x2 * sin, x2 * cos + x1 * sin], axis=-1).astype(x.dtype)


def _gla_chunked(q, k, v, log_a):
    B, H, L, dk = q.shape
    dv = v.shape[-1]
    C = GLA_CHUNK
    n = L // C
    f32 = jnp.float32
    qc = q.astype(f32).reshape(B, H, n, C, dk) * dk ** -0.5
    kc = k.astype(f32).reshape(B, H, n, C, dk)
    vc = v.astype(f32).reshape(B, H, n, C, dv)
    b = jnp.cumsum(log_a.astype(f32).reshape(B, H, n, C, dk), axis=3)
    b_last = b[:, :, :, -1:, :]
    q_dec = qc * jnp.exp(b)
    s = jnp.einsum('bhnid,bhnjd->bhnij', q_dec, kc * jnp.exp(-b))
    causal = jnp.tril(jnp.ones((C, C), dtype=bool))
    o_intra = jnp.einsum('bhnij,bhnje->bhnie', jnp.where(causal, s, 0.0), vc)
    chunk_kv = jnp.einsum('bhnjd,bhnje->bhnde', kc * jnp.exp(b_last - b), vc)
    chunk_decay = jnp.exp(b_last[:, :, :, 0, :])[..., None]

    def step(state, inp):
        kv, dec = inp
        return dec * state + kv, state

    _, prev = lax.scan(step, jnp.zeros((B, H, dk, dv), f32),
                       (jnp.moveaxis(chunk_kv, 2, 0), jnp.moveaxis(chunk_decay, 2, 0)))
    prev = jnp.moveaxis(prev, 0, 2)
    o_inter = jnp.einsum('bhnid,bhnde->bhnie', q_dec, prev)
    return (o_intra + o_inter).reshape(B, H, L, dv).astype(v.dtype)


def _diff_attention(q, k, v, lam):
    B, H, _, L, d = q.shape
    nb = L // ATTN_BLOCK
    q_blocks = jnp.moveaxis(q.reshape(B, H, 2, nb, ATTN_BLOCK, d), 3, 0)
    k_pos = jnp.arange(L)
    scale = d ** -0.5

    def one_block(args):
        q_blk, blk = args
        s = jnp.einsum('bhmqd,bhmkd->bhmqk', q_blk, k, preferred_element_type=jnp.float32) * scale
        q_pos = blk * ATTN_BLOCK + jnp.arange(ATTN_BLOCK)
        s = jnp.where(k_pos[None, :] <= q_pos[:, None], s, -jnp.inf)
        p = jax.nn.softmax(s, axis=-1)
        w = (p[:, :, 0] - lam * p[:, :, 1]).astype(v.dtype)
        return jnp.einsum('bhqk,bhke->bhqe', w, v)

    out = lax.map(one_block, (q_blocks, jnp.arange(nb)))
    return jnp.moveaxis(out, 0, 2).reshape(B, H, L, v.shape[-1])


def _spatial_gating(u, v, w_s, b_s):
    B, L, G, ch = v.shape
    n = L // SGU_CHUNK
    w = jnp.tril(w_s)
    vc = v.reshape(B, n, SGU_CHUNK, G, ch)
    s = jnp.einsum('gij,bnjgc->bnigc', w, vc) + b_s.T[None, None, :, :, None]
    return u * s.reshape(B, L, G, ch)


def _retention_chunked(q, k, v):
    B, H, L, dk = q.shape
    dv = v.shape[-1]
    C = RET_CHUNK
    n = L // C
    f32 = jnp.float32
    log_g = jnp.log(1.0 - 2.0 ** (-5.0 - jnp.arange(H, dtype=f32)))
    qc = q.astype(f32).reshape(B, H, n, C, dk)
    kc = k.astype(f32).reshape(B, H, n, C, dk) * dk ** -0.5
    vc = v.astype(f32).reshape(B, H, n, C, dv)
    idx = jnp.arange(C, dtype=f32)
    rel = idx[:, None] - idx[None, :]
    decay = jnp.where(rel >= 0, jnp.exp(log_g[:, None, None] * jnp.maximum(rel, 0.0)), 0.0)
    s = jnp.einsum('bhnid,bhnjd->bhnij', qc, kc) * decay[None, :, None]
    o_intra = jnp.einsum('bhnij,bhnje->bhnie', s, vc)
    k_dec = jnp.exp(log_g[:, None] * (C - 1.0 - idx))
    chunk_kv = jnp.einsum('bhnjd,bhnje->bhnde', kc * k_dec[None, :, None, :, None], vc)
    chunk_g = jnp.exp(log_g * C)[None, :, None, None]

    def step(state, kv):
        return chunk_g * state + kv, state

    _, prev = lax.scan(step, jnp.zeros((B, H, dk, dv), f32), jnp.moveaxis(chunk_kv, 2, 0))
    prev = jnp.moveaxis(prev, 0, 2)
    q_dec = jnp.exp(log_g[:, None] * (idx + 1.0))
    o_inter = jnp.einsum('bhnid,bhnde->bhnie', qc * q_dec[None, :, None, :, None], prev)
    return (o_intra + o_inter).reshape(B, H, L, dv).astype(v.dtype)


def _moe(h, w_router, b_router, w_in, b_in, w_out, b_out):
    T, D = h.shape
    logits = (h @ w_router + b_router).astype(jnp.float32)
    top_vals, top_idx = lax.top_k(logits, TOP_K)
    gates = jax.nn.softmax(top_vals, axis=-1).astype(h.dtype)
    TK = T * TOP_K
    e_flat = top_idx.reshape(-1)
    tok_flat = jnp.repeat(jnp.arange(T, dtype=jnp.int32), TOP_K)
    g_flat = gates.reshape(-1)
    order = jnp.argsort(e_flat)
    e_sorted, tok_sorted, g_sorted = e_flat[order], tok_flat[order], g_flat[order]
    counts = jnp.bincount(e_flat, length=N_EXPERTS)
    padded = (counts + MOE_BLOCK - 1) // MOE_BLOCK * MOE_BLOCK
    start = jnp.cumsum(counts) - counts
    pad_end = jnp.cumsum(padded)
    pad_start = pad_end - padded
    dest = pad_start[e_sorted] + (jnp.arange(TK) - start[e_sorted])
    n_blocks = (TK + MOE_BLOCK - 1) // MOE_BLOCK + N_EXPERTS
    R = n_blocks * MOE_BLOCK
    row_tok = jnp.full((R,), T, dtype=jnp.int32).at[dest].set(tok_sorted)
    row_gate = jnp.zeros((R,), h.dtype).at[dest].set(g_sorted)
    block_e = jnp.minimum(jnp.searchsorted(pad_end, jnp.arange(n_blocks) * MOE_BLOCK, side='right'),
                          N_EXPERTS - 1)
    h_pad = jnp.concatenate([h, jnp.zeros((1, D), h.dtype)], axis=0)
    xb = h_pad[row_tok].reshape(n_blocks, MOE_BLOCK, D)

    def expert_block(args):
        x_blk, e = args
        z = x_blk @ w_in[e] + b_in[e]
        glu = jnp.minimum(z[:, :D_FF], SWIGLU_LIMIT)
        lin = jnp.clip(z[:, D_FF:], -SWIGLU_LIMIT, SWIGLU_LIMIT)
        a = glu * jax.nn.sigmoid(SWIGLU_ALPHA * glu) * (lin + 1.0)
        return a @ w_out[e] + b_out[e]

    yb = lax.map(expert_block, (xb, block_e)).reshape(R, D)
    y = jnp.zeros((T + 1, D), h.dtype).at[row_tok].add(yb * row_gate[:, None])
    return y[:T]


def _even_mixer(h, positions, layer, w_in, w_gate, b_gate, gla_g, lq1, lk1, lq2, lk2, diff_g, w_out):
    B, L, _ = h.shape
    gq, gk, gv, gr, gg, dq, dk, dv = _split(h @ w_in, EVEN_WIDTHS)
    log_a = jax.nn.log_sigmoid((gr @ w_gate + b_gate).astype(jnp.float32)) / GLA_GATE_NORMALIZER
    o_gla = _gla_chunked(_heads(gq, GLA_HEADS), _heads(gk, GLA_HEADS), _heads(gv, GLA_HEADS),
                         _heads(log_a, GLA_HEADS))
    o_gla = _merge(_rms(o_gla) * gla_g) * jax.nn.silu(gg)
    lam_init = 0.8 - 0.6 * math.exp(-0.3 * layer)
    lam = (jnp.exp(jnp.sum(lq1 * lk1).astype(jnp.float32))
           - jnp.exp(jnp.sum(lq2 * lk2).astype(jnp.float32)) + lam_init)
    q = _rope(dq.reshape(B, L, DIFF_HEADS, 2, DIFF_D).transpose(0, 2, 3, 1, 4), positions)
    k = _rope(dk.reshape(B, L, DIFF_HEADS, 2, DIFF_D).transpose(0, 2, 3, 1, 4), positions)
    o_diff = _diff_attention(q, k, _heads(dv, DIFF_HEADS), lam)
    o_diff = _merge(_rms(o_diff) * diff_g) * (1.0 - lam_init)
    return jnp.concatenate([o_gla, o_diff], axis=-1) @ w_out


def _odd_mixer(h, positions, w_in, ln_g, ln_b, w_s, b_s, w_out):
    B, L, _ = h.shape
    su, sv, rq, rk, rv, rg = _split(h @ w_in, ODD_WIDTHS)
    su = jax.nn.gelu(su, approximate=False)
    sv = _layer_norm(jax.nn.gelu(sv, approximate=False), ln_g, ln_b)
    o_sgu = _spatial_gating(su.reshape(B, L, SGU_GROUPS, SGU_CH), sv.reshape(B, L, SGU_GROUPS, SGU_CH),
                            w_s, b_s).reshape(B, L, SGU_W)
    q = _rope(_heads(rq, RET_HEADS), positions)
    k = _rope(_heads(rk, RET_HEADS), positions)
    o_ret = _merge(_rms(_retention_chunked(q, k, _heads(rv, RET_HEADS)))) * jax.nn.silu(rg)
    return jnp.concatenate([o_sgu, o_ret], axis=-1) @ w_out


def setup_inputs(seed: int = 0) -> dict:
    key = jax.random.key(seed)
    k = jax.random.split(key, 32)
    f32 = jnp.float32
    D = D_MODEL

    def nrm(i, shape, scale):
        return jax.random.normal(k[i], shape, f32) * scale

    return {
        'x': nrm(0, (BATCH, SEQ, D), 1.0),
        'c': nrm(1, (BATCH, D), 1.0),
        'positions': (jnp.arange(SEQ, dtype=jnp.int32)[None, :]
                      + jax.random.randint(k[2], (BATCH, 1), 0, POS_OFFSET_MAX, dtype=jnp.int32)),
        'w_ada': nrm(3, (DEPTH, D, 6 * D), 0.5 * D ** -0.5),
        'b_ada': nrm(4, (DEPTH, 6 * D), 0.02),
        'even_w_in': nrm(5, (N_EVEN, D, EVEN_IN), D ** -0.5),
        'gla_w_gate': nrm(6, (N_EVEN, GLA_RANK, GLA_QK_W), GLA_RANK ** -0.5),
        'gla_b_gate': nrm(7, (N_EVEN, GLA_QK_W), 0.02),
        'gla_norm_g': 1.0 + nrm(8, (N_EVEN, GLA_DV), 0.02),
        'diff_lam_q1': nrm(9, (N_EVEN, DIFF_D), 0.1),
        'diff_lam_k1': nrm(10, (N_EVEN, DIFF_D), 0.1),
        'diff_lam_q2': nrm(11, (N_EVEN, DIFF_D), 0.1),
        'diff_lam_k2': nrm(12, (N_EVEN, DIFF_D), 0.1),
        'diff_norm_g': 1.0 + nrm(13, (N_EVEN, DIFF_DV), 0.02),
        'even_w_out': nrm(14, (N_EVEN, EVEN_OUT, D), EVEN_OUT ** -0.5),
        'odd_w_in': nrm(15, (N_ODD, D, ODD_IN), D ** -0.5),
        'sgu_ln_g': 1.0 + nrm(16, (N_ODD, SGU_W), 0.02),
        'sgu_ln_b': nrm(17, (N_ODD, SGU_W), 0.02),
        'sgu_w': nrm(18, (N_ODD, SGU_GROUPS, SGU_CHUNK, SGU_CHUNK), SGU_CHUNK ** -0.5),
        'sgu_b': 1.0 + nrm(19, (N_ODD, SGU_GROUPS, SGU_CHUNK), 0.02),
        'odd_w_out': nrm(20, (N_ODD, ODD_OUT, D), ODD_OUT ** -0.5),
        'router_w': nrm(21, (DEPTH, D, N_EXPERTS), D ** -0.5),
        'router_b': nrm(22, (DEPTH, N_EXPERTS), 0.01),
        'expert_w_in': nrm(23, (DEPTH, N_EXPERTS, D, 2 * D_FF), D ** -0.5),
        'expert_b_in': nrm(24, (DEPTH, N_EXPERTS, 2 * D_FF), 0.02),
        'expert_w_out': nrm(25, (DEPTH, N_EXPERTS, D_FF, D), D_FF ** -0.5),
        'expert_b_out': nrm(26, (DEPTH, N_EXPERTS, D), 0.02),
        'final_norm_g': 1.0 + nrm(27, (D,), 0.02),
    }


def reference(x, c, positions, w_ada, b_ada, even_w_in, gla_w_gate, gla_b_gate, gla_norm_g,
              diff_lam_q1, diff_lam_k1, diff_lam_q2, diff_lam_k2, diff_norm_g, even_w_out,
              odd_w_in, sgu_ln_g, sgu_ln_b, sgu_w, sgu_b, odd_w_out,
              router_w, router_b, expert_w_in, expert_b_in, expert_w_out, expert_b_out,
              final_norm_g):
    B, L, D = x.shape
    c_act = jax.nn.silu(c)
    for layer in range(DEPTH):
        mod = (c_act @ w_ada[layer] + b_ada[layer])[:, None, :]
        shift1, scale1, gate1, shift2, scale2, gate2 = jnp.split(mod, 6, axis=-1)
        h = _rms(x) * (1.0 + scale1) + shift1
        j = layer // 2
        if layer % 2 == 0:
            y = _even_mixer(h, positions, layer, even_w_in[j], gla_w_gate[j], gla_b_gate[j], gla_norm_g[j],
                            diff_lam_q1[j], diff_lam_k1[j], diff_lam_q2[j], diff_lam_k2[j],
                            diff_norm_g[j], even_w_out[j])
        else:
            y = _odd_mixer(h, positions, odd_w_in[j], sgu_ln_g[j], sgu_ln_b[j], sgu_w[j], sgu_b[j],
                           odd_w_out[j])
        x = x + gate1 * y
        h = _rms(x) * (1.0 + scale2) + shift2
        y = _moe(h.reshape(B * L, D), router_w[layer], router_b[layer], expert_w_in[layer],
                 expert_b_in[layer], expert_w_out[layer], expert_b_out[layer]).reshape(B, L, D)
        x = x + gate2 * y
    return _rms(x) * final_norm_g
```

```python
from contextlib import ExitStack
import math
import threading
import numpy as np
import concourse.bass as bass
import concourse.mybir as mybir
from concourse.bass_utils import run_bass_kernel_spmd

F32 = mybir.dt.float32
BF16 = mybir.dt.bfloat16
I32 = mybir.dt.int32
AF = mybir.ActivationFunctionType
ALU = mybir.AluOpType
AX = mybir.AxisListType

D = 1024
L = 2048
NT = 16
NQ = 4
KC = 8
NE = 32
EPS = 1e-6
N_CORES = 8


class Buf:
    __slots__ = ("name", "w", "r", "excl")

    def __init__(self, name="", excl=False):
        self.name = name
        self.w = None
        self.r = {}
        self.excl = excl


class Sched:
    ENGS = ("pe", "act", "dve", "pool", "sp")
    LIMIT = 30000
    NSLOT = 8

    def __init__(self, nc, stack):
        self.nc = nc
        self.stack = stack
        self.eng = {"pe": nc.tensor, "act": nc.scalar, "dve": nc.vector,
                    "pool": nc.gpsimd, "sp": nc.sync}
        self.ops = []
        self.nops = {e: 0 for e in self.ENGS}
        self.seen = {e: {} for e in self.ENGS}
        self.ndma = {e: 0 for e in self.ENGS}
        self.marked = set()
        self.last_dma = {}
        self.il = None

    def _deps(self, eng, reads, writes, skip_same):
        deps = {}

        def add(tok):
            if tok is None:
                return
            k, i = tok
            if skip_same and k == eng:
                return
            if deps.get(k, -1) < i:
                deps[k] = i
        for b in reads:
            add(b.w)
        for b in writes:
            add(b.w)
            for k, i in b.r.items():
                add((k, i))
        seen = self.seen[eng]
        waits = []
        for k, i in deps.items():
            if seen.get(k, -1) >= i:
                continue
            seen[k] = i
            waits.append((k, i))
            self.marked.add((k, i))
        return waits

    def _update(self, tok, reads, writes):
        k, i = tok
        for b in reads:
            if b.r.get(k, -1) < i:
                b.r[k] = i
        for b in writes:
            b.w = tok
            b.r = {}

    def op(self, eng, fn, reads=(), writes=()):
        il = self.il
        if il is not None:
            il.acquire()
        try:
            return self._op(eng, fn, reads, writes)
        finally:
            if il is not None:
                il.release()

    def _op(self, eng, fn, reads=(), writes=()):
        ex = [b for b in reads if b.excl]
        if ex:
            writes = list(writes) + [b for b in ex if b not in writes]
            reads = [b for b in reads if not b.excl]
        waits = self._deps(eng, reads, writes, eng == "pe")
        idx = self.nops[eng]
        self.nops[eng] += 1
        tok = (eng, idx)
        self.ops.append((eng, fn, waits, "c", eng, idx))
        self._update(tok, reads, writes)
        return tok

    def dma(self, eng, fn, reads=(), writes=()):
        assert self.il is None, "no DMA inside interleaved sections"
        n = self.ndma[eng]
        self.ndma[eng] += 1
        slot = n % self.NSLOT
        key = ("d", eng, slot)
        idx = n // self.NSLOT
        waits = self._deps(eng, reads, writes, False)
        if idx > 0:
            seen = self.seen[eng]
            if seen.get(key, -1) < idx - 1:
                seen[key] = idx - 1
                waits.append((key, idx - 1))
        tok = (key, idx)
        self.last_dma[key] = idx
        self.ops.append((eng, fn, waits, "d", key, idx))
        self._update(tok, reads, writes)
        return tok

    def barrier(self):
        if "nobar" in build_program.stages:
            return
        toks = []
        for e in self.ENGS:
            if self.nops[e] > 0:
                toks.append((e, self.nops[e] - 1))
        for key, idx in self.last_dma.items():
            toks.append((key, idx))
        for e in self.ENGS:
            seen = self.seen[e]
            waits = []
            for (k, i) in toks:
                if seen.get(k, -1) >= i:
                    continue
                seen[k] = i
                waits.append((k, i))
                self.marked.add((k, i))
            if waits:
                self.ops.append((e, None, waits, "w", None, None))

    def wait_all(self, eng, bufs):
        waits = self._deps(eng, bufs, (), False)
        self.ops.append((eng, None, waits, "w", None, None))

    def emit(self):
        nc = self.nc
        sems = {}

        def get_sem(name):
            if name not in sems:
                sems[name] = self.stack.enter_context(nc.semaphore(name))
            return sems[name]

        val = {}
        counters = {e: 0 for e in self.ENGS}
        for (eng, fn, waits, kind, key, idx) in self.ops:
            if kind == "c" and (key, idx) in self.marked:
                c = counters[eng]
                counters[eng] += 1
                val[(key, idx)] = ("s_%s_%d" % (eng, c // self.LIMIT), (c % self.LIMIT) + 1)
        nw = 0
        for (eng, fn, waits, kind, key, idx) in self.ops:
            e = self.eng[eng]
            for (k, i) in waits:
                if isinstance(k, tuple):
                    sname = "d_%s_%d" % (k[1], k[2])
                    v = 16 * (i + 1)
                else:
                    sname, v = val[(k, i)]
                e.wait_ge(get_sem(sname), v)
                nw += 1
            if fn is None:
                continue
            ins = fn(e)
            if kind == "d":
                ins.then_inc(get_sem("d_%s_%d" % (key[1], key[2])), 16)
            elif (key, idx) in self.marked:
                ins.then_inc(get_sem(val[(key, idx)][0]), 1)
        self.nwaits = nw
        return len(sems)


class Interleaver:
    def __init__(self):
        self.cv = threading.Condition()
        self.turn = 0
        self.alive = [True, True]
        self.ids = {}

    def register(self, i):
        self.ids[threading.get_ident()] = i

    def acquire(self):
        me = self.ids[threading.get_ident()]
        with self.cv:
            while self.turn != me:
                self.cv.wait()

    def release(self):
        me = self.ids[threading.get_ident()]
        with self.cv:
            if self.alive[1 - me]:
                self.turn = 1 - me
            self.cv.notify_all()

    def finish(self, i):
        with self.cv:
            self.alive[i] = False
            self.turn = 1 - i
            self.cv.notify_all()


class V:
    __slots__ = ("ap", "buf")

    def __init__(self, ap, buf=None, name=""):
        self.ap = ap
        self.buf = buf if buf is not None else Buf(name)

    def __getitem__(self, k):
        return V(self.ap[k], self.buf)

    def bitcast(self, dt):
        return V(self.ap.bitcast(dt), self.buf)

    def re(self, s, **kw):
        return V(self.ap.rearrange(s, **kw), self.buf)

    def bc(self, axis, shape):
        return V(self.ap.unsqueeze(axis).broadcast_to(shape), self.buf)


def _a(x):
    return x.ap if isinstance(x, V) else x


def _bufs(*xs):
    out = []
    for x in xs:
        if isinstance(x, V) and x.buf not in out:
            out.append(x.buf)
    return out


class K:
    def __init__(self, S):
        self.S = S

    def tt(self, eng, out, a, b, op):
        return self.S.op(eng, lambda e: e.tensor_tensor(out=out.ap, in0=a.ap, in1=b.ap, op=op),
                         reads=_bufs(a, b), writes=_bufs(out))

    def ts(self, eng, out, a, s1, op0, s2=None, op1=None):
        kw = {}
        if op1 is not None:
            kw["op1"] = op1
        return self.S.op(eng, lambda e: e.tensor_scalar(out=out.ap, in0=a.ap, scalar1=_a(s1), scalar2=_a(s2),
                                                        op0=op0, **kw),
                         reads=_bufs(a, s1, s2), writes=_bufs(out))

    def stt(self, out, a, s, b, op0, op1):
        return self.S.op("dve", lambda e: e.scalar_tensor_tensor(out=out.ap, in0=a.ap, scalar=_a(s), in1=b.ap,
                                                                 op0=op0, op1=op1),
                         reads=_bufs(a, s, b), writes=_bufs(out))

    def act(self, out, a, func, scale=1.0, bias=None, accum=None):
        kw = {}
        if bias is not None:
            kw["bias"] = _a(bias)
        if accum is not None:
            kw["accum_out"] = accum.ap
        return self.S.op("act", lambda e: e.activation(out=out.ap, in_=a.ap, func=func, scale=_a(scale), **kw),
                         reads=_bufs(a, scale, bias), writes=_bufs(out, accum))

    def copy(self, eng, out, a):
        if eng == "act":
            return self.S.op(eng, lambda e: e.copy(out=out.ap, in_=a.ap), reads=_bufs(a), writes=_bufs(out))
        return self.S.op(eng, lambda e: e.tensor_copy(out=out.ap, in_=a.ap), reads=_bufs(a), writes=_bufs(out))

    def memset(self, eng, out, val):
        return self.S.op(eng, lambda e: e.memset(out.ap, val), writes=_bufs(out))

    def mm(self, out, lhsT, rhs, start=True, stop=True):
        return self.S.op("pe", lambda e: e.matmul(out.ap, lhsT=lhsT.ap, rhs=rhs.ap, start=start, stop=stop),
                         reads=_bufs(lhsT, rhs), writes=_bufs(out))

    def tr(self, out, a, ident):
        return self.S.op("pe", lambda e: e.transpose(out.ap, a.ap, ident.ap),
                         reads=_bufs(a, ident), writes=_bufs(out))

    def red(self, eng, out, a, op, axis=AX.X):
        return self.S.op(eng, lambda e: e.tensor_reduce(out=out.ap, in_=a.ap, axis=axis, op=op),
                         reads=_bufs(a), writes=_bufs(out))

    def recip(self, out, a):
        return self.S.op("dve", lambda e: e.reciprocal(out=out.ap, in_=a.ap), reads=_bufs(a), writes=_bufs(out))

    def dma(self, eng, out, a, **kw):
        return self.S.dma(eng, lambda e: e.dma_start(out=out.ap, in_=a.ap, **kw),
                          reads=_bufs(a), writes=_bufs(out))


class Arena:
    def __init__(self, ap_f32, nwords):
        self.base = ap_f32
        self.n = nwords
        self.off = 0
        self.peak = 0

    def mark(self):
        return self.off

    def release(self, m):
        self.off = m

    def alloc(self, shape, dtype=F32, name="", parts=128):
        n = int(np.prod(shape))
        esz = 4 if dtype in (F32, I32) else 2
        words = (n * esz + 3) // 4
        assert self.off + words <= self.n, ("SBUF arena overflow", name, self.off, words, self.n)
        ap = self.base[0:parts, self.off:self.off + words]
        self.off += words
        self.peak = max(self.peak, self.off)
        if dtype != F32:
            ap = ap.bitcast(dtype)
        ap = ap[:, 0:n]
        if len(shape) > 1:
            names = " ".join("d%d" % i for i in range(len(shape)))
            kw = {"d%d" % i: int(s) for i, s in enumerate(shape)}
            ap = ap.rearrange("p (%s) -> p %s" % (names, names), **kw)
        return V(ap, Buf(name))


class PsPool:
    def __init__(self, banks):
        self.banks = banks
        self.rot = list(range(len(banks)))
        self.i = 0
        self.tl = {}

    def set_rot(self, idxs):
        self.rot = list(idxs)
        self.i = 0

    def next(self):
        st = self.tl.get(threading.get_ident())
        if st is not None:
            b = self.banks[st[0][st[1] % len(st[0])]]
            st[1] += 1
            return b
        b = self.banks[self.rot[self.i % len(self.rot)]]
        self.i += 1
        return b

    def fixed(self, i):
        return self.banks[i]


ARENA_WORDS = 52800


def build_program(layers=(0, 1), do_final=True, dbg=None):
    nc = bass.Bass("TRN2", target_bir_lowering=False)

    stages = build_program.stages
    in_names = []

    def din(name, shape, dt=F32, used=True):
        if not used:
            return None
        in_names.append(name)
        return V(nc.dram_tensor(name, list(shape), dt, kind="ExternalInput").ap(), name=name)

    has_mix0 = (0 in layers) and ("mix" in stages)
    has_mix1 = (1 in layers) and ("mix" in stages)
    has_moe = len(layers) > 0 and ("moe" in stages)
    has_mod = len(layers) > 0 and ("nomod" not in stages)
    d_x = din("x", [L, D])
    d_cT = din("cT", [128, KC])
    d_pos = din("posT", [128, NT], I32)
    d_invf = din("invf", [128, 32])
    d_wada = din("w_ada", [2, D, 6 * D], used=has_mod)
    d_bada = din("b_adaT", [128, 2, 48])
    d_ewin = din("even_w_in", [D, 3088], used=has_mix0)
    d_wg = din("gla_w_gate", [16, 256], used=has_mix0)
    d_bg = din("gla_b_gate", [1, 256], used=has_mix0)
    d_glag = din("gla_norm_g", [1, 128], used=has_mix0)
    d_lam = din("lam_in", [1, 256], used=has_mix0)
    d_diffg = din("diff_norm_g", [1, 128], used=has_mix0)
    d_ewout = din("even_w_out", [D, D], used=has_mix0)
    d_owin = din("odd_w_in", [D, 2560], used=has_mix1)
    d_lng = din("sgu_ln_g", [1, 512], used=has_mix1)
    d_lnb = din("sgu_ln_b", [1, 512], used=has_mix1)
    d_swT = din("sgu_wT", [4, 128, 128], used=has_mix1)
    d_sbT = din("sgu_bT", [128, 4], used=has_mix1)
    d_owout = din("odd_w_out", [D, D], used=has_mix1)
    d_rw = din("router_w", [2, D, NE], used=has_moe)
    d_rb = din("router_b", [2, 1, NE], used=has_moe)
    d_ewi = din("expert_w_in", [2, NE, D, 2 * D], used=has_moe)
    d_ebi = din("expert_b_inT", [2, 128, NE, 16], used=has_moe)
    d_ewo = din("expert_w_out", [2, NE, D, D], used=has_moe)
    d_ebo = din("expert_b_out", [2, NE, D], used=has_moe)
    d_fg = din("final_gT", [128, KC])
    build_program.in_names = in_names
    d_out = V(nc.dram_tensor("yout", [L, D], F32, kind="ExternalOutput").ap(), name="out")
    d_gtb = [V(nc.dram_tensor("gtb%d" % l, [NE, L], BF16, kind="Internal").ap(), name="gtb%d" % l) for l in range(2)]
    d_dbg = None
    if dbg is not None:
        d_dbg = V(nc.dram_tensor("dbg", [128, dbg], F32, kind="ExternalOutput").ap(), name="dbg")

    with ExitStack() as st:
        S = Sched(nc, st)
        k = K(S)
        big = st.enter_context(nc.sbuf_tensor("arena", [128, ARENA_WORDS], F32))
        A = Arena(big, ARENA_WORDS)
        banks = [V(st.enter_context(nc.psum_tensor("ps%d" % i, [128, 512], F32))[:, :], Buf("ps%d" % i, excl=True))
                 for i in range(8)]
        PS = PsPool(banks)

        def interleave(fA, rotA, fB, rotB):
            il = Interleaver()
            errs = []

            def wrap(i, f, rot):
                il.register(i)
                PS.tl[threading.get_ident()] = [list(rot), 0]
                try:
                    if f is not None:
                        f()
                except BaseException as ex:
                    errs.append(ex)
                finally:
                    PS.tl.pop(threading.get_ident(), None)
                    il.finish(i)
            S.il = il
            ts = [threading.Thread(target=wrap, args=(0, fA, rotA)),
                  threading.Thread(target=wrap, args=(1, fB, rotB))]
            for t in ts:
                t.start()
            for t in ts:
                t.join()
            S.il = None
            if errs:
                raise errs[0]

        xT_all = A.alloc([KC, L], F32, "xT")
        xT = [[V(xT_all.ap[:, c, q * 512:(q + 1) * 512], Buf("xT%d_%d" % (c, q))) for q in range(NQ)]
              for c in range(KC)]
        hT_all = A.alloc([KC, L], BF16, "hT")
        hT = [V(hT_all.ap[:, :, q * 512:(q + 1) * 512], Buf("hT%d" % q)) for q in range(NQ)]

        def hT_tile(tt):
            q, r = divmod(tt, 4)
            return hT[q][:, :, r * 128:(r + 1) * 128]

        ident = A.alloc([128], F32, "ident")
        identb = A.alloc([128], BF16, "identb")
        tri = A.alloc([128], F32, "tri")
        trib = A.alloc([128], BF16, "trib")
        tri16 = A.alloc([128], F32, "tri16")
        ones = A.alloc([128], F32, "ones")
        ones16 = A.alloc([128], F32, "ones16")
        mhalf = A.alloc([8], F32, "mhalf")
        mod = A.alloc([2, 48], F32, "mod")
        fgT = A.alloc([KC], F32, "fgT")
        cosT = A.alloc([NT, 32], F32, "cos")
        sinT = A.alloc([NT, 32], F32, "sin")

        io = A.alloc([128], I32, "io")
        S.op("pool", lambda e: e.iota(io.ap, pattern=[[1, 128]], base=0, channel_multiplier=-1),
             writes=[io.buf])
        iof = A.alloc([128], F32, "iof")
        k.copy("dve", iof, io)
        k.ts("dve", tri, iof, 0.0, ALU.is_ge)
        k.ts("dve", ident, iof, 0.0, ALU.is_equal)
        k.copy("dve", identb, ident)
        k.copy("dve", trib, tri)
        k.ts("dve", tri16, tri, 1.0 / 16.0, ALU.mult)
        k.memset("dve", ones, 1.0)
        k.memset("dve", ones16, 1.0 / 16.0)
        k.memset("dve", mhalf, -0.5)
        k.dma("sp", fgT, d_fg)

        dbg_off = [0]

        def dump(v, ncols, parts=128):
            if d_dbg is None:
                return
            m = A.mark()
            t = A.alloc([ncols], F32, "dbgt")
            k.copy("dve", t[0:parts, :], v)
            k.dma("sp", d_dbg[0:parts, dbg_off[0]:dbg_off[0] + ncols], t[0:parts, :])
            S.barrier()
            A.release(m)
            dbg_off[0] += ncols

        def prologue():
            m0 = A.mark()
            PS.set_rot(range(8))
            cT = A.alloc([KC], F32, "cT")
            k.dma("sp", cT, d_cT)
            cact = A.alloc([KC], F32, "cact")
            k.act(cact, cT, AF.Silu)
            badaT = A.alloc([2, 48], F32, "badaT")
            k.dma("sp", badaT, d_bada)
            wa = [A.alloc([KC, 512], F32, "wa%d" % i) for i in range(2)]
            nld = 0
            for l in range(2):
                if l not in layers or "nomod" in build_program.stages:
                    continue
                pm = PS.next()
                wv = d_wada[l].re("(kc p) n -> p kc n", p=128)
                for g in range(12):
                    w = wa[nld % 2]
                    nld += 1
                    k.dma("sp", w, wv[:, :, g * 512:(g + 1) * 512])
                    for jj in range(4):
                        j = g * 4 + jj
                        for kc in range(KC):
                            k.mm(pm[:, j:j + 1], w[:, kc, jj * 128:(jj + 1) * 128], cact[:, kc:kc + 1],
                                 start=(kc == 0), stop=(kc == KC - 1))
                k.tt("dve", mod[:, l, :], pm[:, 0:48], badaT[:, l, :], ALU.add)
                k.ts("dve", mod[:, l, 8:16], mod[:, l, 8:16], 1.0, ALU.add)
                k.ts("dve", mod[:, l, 32:40], mod[:, l, 32:40], 1.0, ALU.add)
            xin = [A.alloc([D], F32, "xin%d" % i) for i in range(2)]
            for tt in range(NT if "nox" not in build_program.stages else 1):
                xi = xin[tt % 2]
                k.dma("sp", xi, d_x[tt * 128:(tt + 1) * 128, :])
                q, r = divmod(tt, 4)
                for half in range(2):
                    pb = PS.next()
                    for cc in range(4):
                        c = half * 4 + cc
                        k.tr(pb[:, cc * 128:(cc + 1) * 128], xi[:, c * 128:(c + 1) * 128], ident)
                    for cc in range(4):
                        c = half * 4 + cc
                        k.copy("act" if cc % 2 else "dve", xT[c][q][:, r * 128:(r + 1) * 128],
                               pb[:, cc * 128:(cc + 1) * 128])
            posi = A.alloc([NT], I32, "posi")
            k.dma("sp", posi, d_pos)
            posf = A.alloc([NT], F32, "posf")
            k.copy("dve", posf, posi)
            invf = A.alloc([32], F32, "invf")
            k.dma("sp", invf, d_invf)
            ang = A.alloc([NT, 32], F32, "ang")
            if "noang" not in build_program.stages:
                k.tt("dve", ang, posf.bc(2, [128, NT, 32]), invf.bc(1, [128, NT, 32]), ALU.mult)
            C1 = 6.28125
            C2 = 2.0 * math.pi - C1
            TWO_PI = 2.0 * math.pi

            def reduce_sin(dst, src_ang, shift):
                a = A.alloc([NT, 32], F32, "rs_a")
                if shift != 0.0:
                    k.ts("dve", a, src_ang, shift, ALU.add)
                else:
                    k.copy("dve", a, src_ang)
                nf = A.alloc([NT, 32], F32, "rs_n")
                k.ts("dve", nf, a, 1.0 / TWO_PI, ALU.mult)
                ni = A.alloc([NT, 32], I32, "rs_ni")
                k.copy("dve", ni, nf)
                k.copy("dve", nf, ni)
                r = A.alloc([NT, 32], F32, "rs_r")
                k.stt(r, nf, -C1, a, ALU.mult, ALU.add)
                k.stt(r, nf, -C2, r, ALU.mult, ALU.add)
                m1 = A.alloc([NT, 32], F32, "rs_m")
                k.ts("dve", m1, r, math.pi, ALU.is_gt)
                k.stt(r, m1, -TWO_PI, r, ALU.mult, ALU.add)
                k.ts("dve", m1, r, -math.pi, ALU.is_lt)
                k.stt(r, m1, TWO_PI, r, ALU.mult, ALU.add)
                k.ts("dve", r, r, math.pi, ALU.min, -math.pi, ALU.max)
                k.act(dst, r, AF.Sin)

            if "norope" not in build_program.stages:
                reduce_sin(sinT, ang, 0.0)
                reduce_sin(cosT, ang, math.pi / 2.0)
            S.barrier()
            A.release(m0)

        def norm_alloc():
            return dict(sq=[A.alloc([512], F32, "sq%d" % i) for i in range(2)],
                        sd=A.alloc([512], F32, "sd"), rstd=A.alloc([512], F32, "rstd"),
                        tmp=[A.alloc([512], F32, "ntmp%d" % i) for i in range(2)])

        def norm_quad(q, scale_cols, shift_cols, out_bf, out_f32=None, nt=None):
            sq, sd, rstd, tmp = nt["sq"], nt["sd"], nt["rstd"], nt["tmp"]
            pss = PS.next()
            for c in range(KC):
                k.act(sq[c % 2], xT[c][q], AF.Square)
                k.mm(pss, ones, sq[c % 2], start=(c == 0), stop=(c == KC - 1))
            k.act(sd, pss, AF.Sqrt, scale=1.0 / D, bias=EPS)
            k.recip(rstd, sd)
            for c in range(KC):
                t = tmp[c % 2]
                k.tt("dve" if c % 2 else "pool", t, xT[c][q], rstd, ALU.mult)
                if out_f32 is not None:
                    k.act(out_f32[:, c, :], t, AF.Identity, scale=scale_cols[c], bias=shift_cols[c])
                    k.copy("pool", out_bf[:, c, :], out_f32[:, c, :])
                else:
                    k.act(out_bf[:, c, :], t, AF.Identity, scale=scale_cols[c], bias=shift_cols[c])

        def norm_all(scale_cols, shift_cols):
            mN = A.mark()
            nt = norm_alloc()
            for q in range(NQ):
                norm_quad(q, scale_cols, shift_cols, hT[q], nt=nt)
            S.barrier()
            A.release(mN)

        def mod_cols(l, j):
            return [mod[:, l, j * 8 + c: j * 8 + c + 1] for c in range(KC)]

        def rope(dst, src, tt, tmp4):
            sv = src.re("p (g two f) -> p g two f", g=8, two=2)
            dv = dst.re("p (g two f) -> p g two f", g=8, two=2)
            x1, x2 = sv[:, :, 0, :], sv[:, :, 1, :]
            cs = cosT[:, tt, :].bc(1, [128, 8, 32])
            sn = sinT[:, tt, :].bc(1, [128, 8, 32])
            t1, t2, t3, t4 = [t.re("p (g f) -> p g f", g=8) for t in tmp4]
            k.tt("dve", t1, x1, cs, ALU.mult)
            k.tt("pool", t2, x2, sn, ALU.mult)
            k.tt("dve", dv[:, :, 0, :], t1, t2, ALU.subtract)
            k.tt("pool", t3, x2, cs, ALU.mult)
            k.tt("dve", t4, x1, sn, ALU.mult)
            k.tt("pool", dv[:, :, 1, :], t3, t4, ALU.add)

        def linattn_core(tt, q_dec, k_dec, k2, v_sb, dec, Sf, Sb, qkT_sb, sT_sb):
            QT = PS.next()
            QTb = QT.bitcast(BF16).re("p (g f) -> p g f", g=8)
            for h in range(4):
                k.tr(QTb[0:64, h, :], q_dec[:, h * 64:(h + 1) * 64], identb)
                k.tr(QTb[0:64, 4 + h, :], k_dec[:, h * 64:(h + 1) * 64], identb)
            k.copy("act", qkT_sb[0:64], QTb[0:64])
            ST = PS.next()
            for h in range(4):
                k.mm(ST[:, h * 128:(h + 1) * 128], qkT_sb[0:64, 4 + h, :], qkT_sb[0:64, h, :])
            k.tt("dve", sT_sb, ST.re("p (h f) -> p h f", h=4), tri.bc(1, [128, 4, 128]), ALU.mult)
            O = PS.next()
            for h in range(4):
                k.mm(O[:, h * 128:(h + 1) * 128], sT_sb[:, h, :], v_sb[:, h * 128:(h + 1) * 128],
                     start=True, stop=(tt == 0))
                if tt > 0:
                    k.mm(O[:, h * 128:(h + 1) * 128], qkT_sb[0:64, h, :], Sb[0:64, h, :],
                         start=False, stop=True)
            if tt < NT - 1:
                KV = PS.next()
                for h in range(4):
                    k.mm(KV[0:64, h * 128:(h + 1) * 128], k2[:, h * 64:(h + 1) * 64],
                         v_sb[:, h * 128:(h + 1) * 128])
                for h in range(4):
                    if tt == 0:
                        k.copy("dve", Sf[0:64, h, :], KV[0:64, h * 128:(h + 1) * 128])
                    else:
                        k.stt(Sf[0:64, h, :], Sf[0:64, h, :], dec[0:64, h:h + 1],
                              KV[0:64, h * 128:(h + 1) * 128], ALU.mult, ALU.add)
                k.copy("act", Sb[0:64], Sf[0:64])
            return O

        def head_rms(O, o_sb, osq, ssq, rs):
            k.copy("act", o_sb, O)
            k.tt("pool", osq, o_sb, o_sb, ALU.mult)
            k.red("dve", ssq, osq.re("p (h f) -> p h f", h=4), ALU.add)
            k.ts("dve", ssq, ssq, 1.0 / 128.0, ALU.mult, EPS, ALU.add)
            k.tt("pool", rs, ssq, mhalf[:, 0:4], ALU.pow)

        def out_proj(tt, o_tok, nk, wo_sb, kc0, gate_cols, oT_sb):
            q, r = divmod(tt, 4)
            OT = PS.next()
            OTb = OT.bitcast(BF16)
            for kc in range(nk):
                k.tr(OTb[:, kc * 128:(kc + 1) * 128], o_tok[:, kc * 128:(kc + 1) * 128], identb)
            k.copy("act", oT_sb[:, 0:nk, :], OTb[:, 0:nk * 128].re("p (g f) -> p g f", g=nk))
            for half in range(2):
                Y = PS.next()
                for dd in range(4):
                    dc = half * 4 + dd
                    for kc in range(nk):
                        k.mm(Y[:, dd * 128:(dd + 1) * 128], wo_sb[:, kc0 + kc, dc * 128:(dc + 1) * 128],
                             oT_sb[:, kc, :], start=(kc == 0), stop=(kc == nk - 1))
                for dd in range(4):
                    dc = half * 4 + dd
                    xs = xT[dc][q][:, r * 128:(r + 1) * 128]
                    k.stt(xs, Y[:, dd * 128:(dd + 1) * 128], gate_cols[dc], xs, ALU.mult, ALU.add)

        def load_bc_row(dst, dsrc, n):
            k.dma("sp", dst, V(dsrc.ap.partition_broadcast(128).rearrange("p o n -> p (o n)"), dsrc.buf))

        def even_mixer(l):
            gate1 = mod_cols(l, 2)
            mE = A.mark()
            PS.set_rot(range(8))
            norm_all(mod_cols(l, 1), mod_cols(l, 0))
            wdv = d_ewin.re("(kc p) n -> p kc n", p=128)
            mB = A.mark()
            NTB = 0 if "nogla" in build_program.stages else NT
            wa_ = A.alloc([KC, 1552], BF16, "wga")
            for (c0, c1) in ((0, 512), (512, 1024), (1024, 1552)):
                k.dma("pool", wa_[:, :, c0:c1], wdv[:, :, c0:c1])
            wo = A.alloc([4, D], BF16, "wo")
            k.dma("pool", wo, d_ewout.re("(kc p) n -> p kc n", p=128)[:, 0:4, :])
            wg = A.alloc([256], F32, "wg")
            k.dma("sp", wg[0:16, :], d_wg)
            bg = A.alloc([256], F32, "bg")
            k.dma("sp", bg[0:1, :], d_bg)
            ggb = A.alloc([128], F32, "ggb")
            load_bc_row(ggb, d_glag, 128)
            Sf = A.alloc([4, 128], F32, "Sf")
            Sb = A.alloc([4, 128], BF16, "Sb")
            grT = A.alloc([128], F32, "grT")
            ax = A.alloc([256], F32, "ax")
            ln_ = A.alloc([256], F32, "ln")
            la = A.alloc([256], F32, "la")
            b_sb = A.alloc([256], F32, "b_sb")
            eb = A.alloc([256], F32, "eb")
            enb = A.alloc([256], F32, "enb")
            e2 = A.alloc([256], F32, "e2")
            dec = A.alloc([4], F32, "dec")
            q_dec = A.alloc([256], BF16, "q_dec")
            k_dec = A.alloc([256], BF16, "k_dec")
            k2 = A.alloc([256], BF16, "k2")
            v_sb = A.alloc([512], BF16, "v_sb")
            qkT_sb = A.alloc([8, 128], BF16, "qkT_sb")
            sT_sb = A.alloc([4, 128], BF16, "sT_sb")
            o_sb = A.alloc([512], F32, "o_sb")
            osq = A.alloc([512], F32, "osq")
            ssq = A.alloc([4], F32, "ssq")
            rs = A.alloc([4], F32, "rs")
            sg = A.alloc([512], F32, "sg")
            o_tok = A.alloc([512], BF16, "o_tok")
            oT_sb = A.alloc([8, 128], BF16, "oT_sb")
            hand = [dict(q_dec=A.alloc([256], BF16, "q_dec%d" % i), k_dec=A.alloc([256], BF16, "k_dec%d" % i),
                         k2=A.alloc([256], BF16, "k2%d" % i), v_sb=A.alloc([512], BF16, "v_sb%d" % i),
                         dec=A.alloc([4], F32, "dec%d" % i), sg=A.alloc([512], F32, "sg%d" % i)) for i in range(2)]

            def gla_A(tt):
                hd = hand[tt % 2]
                q_dec, k_dec, k2, v_sb, dec, sg = hd["q_dec"], hd["k_dec"], hd["k2"], hd["v_sb"], hd["dec"], hd["sg"]
                h_t = hT_tile(tt)
                ZA = PS.fixed(0)
                ZB, ZG, ZR = PS.next(), PS.next(), PS.next()
                for (Zp, c0) in ((ZA, 0), (ZB, 512), (ZG, 1040)):
                    for kc in range(KC):
                        k.mm(Zp, h_t[:, kc, :], wa_[:, kc, c0:c0 + 512], start=(kc == 0), stop=(kc == KC - 1))
                for kc in range(KC):
                    k.mm(ZR[0:16, 0:128], wa_[:, kc, 1024:1040], h_t[:, kc, :],
                         start=(kc == 0), stop=(kc == KC - 1))
                k.copy("act", grT[0:16, :], ZR[0:16, 0:128])
                k.act(sg, ZG, AF.Silu)
                k.copy("act", v_sb, ZB)
                GP = PS.next()
                k.mm(GP[:, 0:256], grT[0:16, :], wg[0:16, :], start=True, stop=False)
                k.mm(GP[:, 0:256], ones[0:1, :], bg[0:1, :], start=False, stop=True)
                k.act(ax, GP[:, 0:256], AF.Abs)
                k.act(ax, ax, AF.Exp, scale=-1.0)
                k.act(ln_, ax, AF.Ln, bias=1.0)
                k.ts("dve", la, GP[:, 0:256], 0.0, ALU.min)
                k.tt("dve", la, la, ln_, ALU.subtract)
                B2 = PS.next()
                k.mm(B2[:, 0:256], tri16, la)
                k.mm(B2[:, 256:512], ones16, la)
                DC = PS.next()
                for h in range(4):
                    k.mm(DC[0:64, h:h + 1], la[:, h * 64:(h + 1) * 64], ones16[:, 0:1])
                k.copy("dve", b_sb, B2[:, 0:256])
                k.act(eb, B2[:, 0:256], AF.Exp)
                k.act(enb, B2[:, 0:256], AF.Exp, scale=-1.0)
                k.tt("dve", e2, B2[:, 256:512], b_sb, ALU.subtract)
                k.act(e2, e2, AF.Exp)
                k.act(dec[0:64, :], DC[0:64, 0:4], AF.Exp)
                k.stt(q_dec, ZA[:, 0:256], 0.125, eb, ALU.mult, ALU.mult)
                k.tt("dve", k_dec, ZA[:, 256:512], enb, ALU.mult)
                k.tt("dve", k2, ZA[:, 256:512], e2, ALU.mult)
                k.tt("pool", sg.re("p (h f) -> p h f", h=4), sg.re("p (h f) -> p h f", h=4),
                     ggb.bc(1, [128, 4, 128]), ALU.mult)

            def gla_B(tt):
                hd = hand[tt % 2]
                q_dec, k_dec, k2, v_sb, dec, sg = hd["q_dec"], hd["k_dec"], hd["k2"], hd["v_sb"], hd["dec"], hd["sg"]
                O = linattn_core(tt, q_dec, k_dec, k2, v_sb, dec, Sf, Sb, qkT_sb, sT_sb)
                head_rms(O, o_sb, osq, ssq, rs)
                if d_dbg is not None and tt == build_program.dbg_tile:
                    dump(la, 256); dump(eb, 256); dump(q_dec, 256); dump(k_dec, 256)
                    dump(sT_sb.re("p h f -> p (h f)"), 512); dump(o_sb, 512); dump(v_sb, 512)
                k.tt("dve", o_sb.re("p (h f) -> p h f", h=4), o_sb.re("p (h f) -> p h f", h=4),
                     rs.bc(2, [128, 4, 128]), ALU.mult)
                k.tt("pool", o_tok, o_sb, sg, ALU.mult)
                if d_dbg is not None and tt == build_program.dbg_tile:
                    dump(o_tok, 512)
                out_proj(tt, o_tok, 4, wo, 0, gate1, oT_sb)

            if NTB:
                interleave(lambda: gla_A(0), [1, 2, 3], None, [4, 5, 6, 7])
            for tt in range(NTB):
                interleave((lambda t=tt: gla_A(t + 1)) if tt + 1 < NTB else None, [1, 2, 3],
                           lambda t=tt: gla_B(t), [4, 5, 6, 7])
            S.barrier()
            A.release(mB)
            qT = A.alloc([4, L], BF16, "qT")
            kT = A.alloc([4, L], BF16, "kT")
            vA = A.alloc([NT, 4, 129], BF16, "vA")
            qmax = A.alloc([8], F32, "qmax")
            kmax = A.alloc([8], F32, "kmax")
            k.memset("dve", qmax, 0.0)
            k.memset("dve", kmax, 0.0)
            k.memset("pool", vA[:, :, :, 128:129], 1.0)
            mA = A.mark()
            wd = A.alloc([KC, 1536], BF16, "wd")
            wdv = d_ewin.re("(kc p) n -> p kc n", p=128)
            for g in range(3):
                k.dma("pool", wd[:, :, g * 512:(g + 1) * 512], wdv[:, :, 1552 + g * 512:1552 + (g + 1) * 512])
            q_sb = A.alloc([512], F32, "q_sb")
            k_sb = A.alloc([512], F32, "k_sb")
            q_rot = A.alloc([512], BF16, "q_rot")
            k_rot = A.alloc([512], BF16, "k_rot")
            rt = [A.alloc([256], F32, "rt%d" % i) for i in range(4)]
            sqt = A.alloc([512], F32, "sqt")
            nrm = A.alloc([8], F32, "nrm")
            for tt in range(NT):
                h_t = hT_tile(tt)
                Z = [PS.next() for _ in range(3)]
                for g in range(3):
                    for kc in range(KC):
                        k.mm(Z[g], h_t[:, kc, :], wd[:, kc, g * 512:(g + 1) * 512],
                             start=(kc == 0), stop=(kc == KC - 1))
                k.copy("act", vA[:, tt, :, 0:128], Z[2].re("p (h f) -> p h f", h=4))
                for (zz, sb, rot, dstT, mx) in ((Z[0], q_sb, q_rot, qT, qmax), (Z[1], k_sb, k_rot, kT, kmax)):
                    k.copy("act", sb, zz)
                    k.tt("pool", sqt, sb, sb, ALU.mult)
                    k.red("dve", nrm, sqt.re("p (g f) -> p g f", g=8), ALU.add)
                    k.tt("dve", mx, mx, nrm, ALU.max)
                    rope(rot, sb, tt, rt)
                    TP = PS.next()
                    TPb = TP.bitcast(BF16)
                    for h in range(4):
                        k.tr(TPb[:, h * 128:(h + 1) * 128], rot[:, h * 128:(h + 1) * 128], identb)
                    k.copy("act", dstT[:, :, tt * 128:(tt + 1) * 128],
                           TPb[:, 0:512].re("p (h f) -> p h f", h=4))
            S.barrier()
            A.release(mA)
            wo2 = A.alloc([4, D], BF16, "wo2")
            k.dma("pool", wo2, d_ewout.re("(kc p) n -> p kc n", p=128)[:, 4:8, :])
            dgb = A.alloc([128], F32, "dgb")
            load_bc_row(dgb, d_diffg, 128)
            lam_init = 0.8 - 0.6 * math.exp(-0.3 * l)
            k.ts("dve", dgb, dgb, 1.0 - lam_init, ALU.mult)
            lin = A.alloc([256], F32, "lin")
            k.dma("sp", lin[0:1, :], d_lam)
            lp = A.alloc([128], F32, "lp")
            k.tt("dve", lp[0:1, :], lin[0:1, 0:128], lin[0:1, 128:256], ALU.mult)
            ls = A.alloc([2], F32, "ls")
            k.red("dve", ls[0:1, :], lp[0:1, :].re("p (g f) -> p g f", g=2), ALU.add)
            k.act(ls[0:1, :], ls[0:1, :], AF.Exp)
            nl = A.alloc([1], F32, "nl")
            k.tt("dve", nl[0:1, :], ls[0:1, 1:2], ls[0:1, 0:1], ALU.subtract)
            k.ts("dve", nl[0:1, :], nl[0:1, :], -lam_init, ALU.add)
            PL = PS.next()
            k.mm(PL[:, 0:1], ones[0:1, :], nl[0:1, 0:1])
            neglam = A.alloc([1], F32, "neglam")
            k.copy("dve", neglam, PL[:, 0:1])
            PQ = PS.next()
            k.tr(PQ[0:8, 0:128], qmax, ident)
            k.tr(PQ[0:8, 128:256], kmax, ident)
            m2 = A.alloc([2], F32, "m2")
            k.red("dve", m2[0:8, :], PQ[0:8, 0:256].re("p (g f) -> p g f", g=2), ALU.max)
            mm_ = A.alloc([1], F32, "mm_")
            k.tt("dve", mm_[0:8, :], m2[0:8, 0:1], m2[0:8, 1:2], ALU.mult)
            k.act(mm_[0:8, :], mm_[0:8, :], AF.Sqrt)
            dg = A.alloc([8], F32, "dg")
            k.ts("dve", dg[0:8, :], ident[0:8, 0:8], mm_[0:8, 0:1], ALU.mult, -0.125, ALU.mult)
            PM = PS.next()
            k.mm(PM[:, 0:8], ones[0:8, :], dg[0:8, :])
            negM = A.alloc([8], F32, "negM")
            k.copy("dve", negM, PM[:, 0:8])
            PT = [A.alloc([512], BF16, "PT%d" % i) for i in range(3)]
            oa = A.alloc([128], F32, "oa")
            od = A.alloc([128], F32, "od")
            r01 = A.alloc([2], F32, "r01")
            dsq = A.alloc([128], F32, "dsq")
            dss = A.alloc([1], F32, "dss")
            o_tok2 = A.alloc([512], BF16, "o_tok2")
            oT2 = A.alloc([8, 128], BF16, "oT2")
            PS.set_rot([4, 5, 6, 7])
            accs = [PS.fixed(0), PS.fixed(1), PS.fixed(2), PS.fixed(3)]
            npt = 0
            pend_pv = []

            def flush_pv():
                while pend_pv:
                    pend_pv.pop(0)()

            for i in range(0 if "nodiff" in build_program.stages else NT):
                for h in range(4):
                    acc = accs[(h % 2) * 2:(h % 2) * 2 + 2]
                    for m in range(2):
                        hm = h * 2 + m
                        for jb in range(0, i + 1, 4):
                            nj = min(4, i + 1 - jb)
                            SB = PS.next()
                            for jj in range(nj):
                                j = jb + jj
                                k.mm(SB[:, jj * 128:(jj + 1) * 128],
                                     kT[m * 64:(m + 1) * 64, h, j * 128:(j + 1) * 128],
                                     qT[m * 64:(m + 1) * 64, h, i * 128:(i + 1) * 128])
                            flush_pv()
                            P = PT[npt % 3]
                            npt += 1
                            k.act(P[:, 0:nj * 128], SB[:, 0:nj * 128], AF.Exp, scale=0.125,
                                  bias=negM[:, hm:hm + 1])
                            if jb + nj == i + 1:
                                dsl = P[:, (nj - 1) * 128:nj * 128]
                                k.tt("pool", dsl, dsl, trib, ALU.mult)

                            def pv(P=P, nj=nj, jb=jb, m=m, h=h, i=i, acc=acc):
                                for jj in range(nj):
                                    j = jb + jj
                                    k.mm(acc[m][:, 0:129], P[:, jj * 128:(jj + 1) * 128], vA[:, j, h, :],
                                         start=(j == 0), stop=(j == i))
                            pend_pv.append(pv)
                    flush_pv()
                    k.recip(r01[:, 0:1], acc[0][:, 128:129])
                    k.recip(r01[:, 1:2], acc[1][:, 128:129])
                    k.tt("dve", r01[:, 1:2], r01[:, 1:2], neglam, ALU.mult)
                    k.ts("dve", oa, acc[0][:, 0:128], r01[:, 0:1], ALU.mult)
                    k.stt(od, acc[1][:, 0:128], r01[:, 1:2], oa, ALU.mult, ALU.add)
                    k.tt("pool", dsq, od, od, ALU.mult)
                    k.red("dve", dss, dsq, ALU.add)
                    k.ts("dve", dss, dss, 1.0 / 128.0, ALU.mult, EPS, ALU.add)
                    k.tt("pool", dss, dss, mhalf[:, 0:1], ALU.pow)
                    k.stt(o_tok2[:, h * 128:(h + 1) * 128], od, dss[:, 0:1], dgb, ALU.mult, ALU.mult)
                out_proj(i, o_tok2, 4, wo2, 0, gate1, oT2)
            S.barrier()
            PS.set_rot(range(8))
            A.release(mE)

        def odd_mixer(l):
            gate1 = mod_cols(l, 2)
            mO = A.mark()
            PS.set_rot(range(8))
            norm_all(mod_cols(l, 1), mod_cols(l, 0))
            wi = A.alloc([KC, 2560], BF16, "wi")
            wv = d_owin.re("(kc p) n -> p kc n", p=128)
            for g in range(5):
                k.dma("pool", wi[:, :, g * 512:(g + 1) * 512], wv[:, :, g * 512:(g + 1) * 512])
            wo = A.alloc([KC, D], BF16, "wo")
            k.dma("pool", wo, d_owout.re("(kc p) n -> p kc n", p=128))
            lngb = A.alloc([512], F32, "lngb")
            lnbb = A.alloc([512], F32, "lnbb")
            load_bc_row(lngb, d_lng, 512)
            load_bc_row(lnbb, d_lnb, 512)
            wsb = A.alloc([4, 128], BF16, "wsb")
            mW = A.mark()
            wsf = A.alloc([4, 128], F32, "wsf")
            k.dma("sp", wsf, d_swT.re("g j i -> j g i"))
            k.tt("dve", wsb, wsf, tri.bc(1, [128, 4, 128]), ALU.mult)
            S.barrier()
            A.release(mW)
            bsT = A.alloc([4], F32, "bsT")
            k.dma("sp", bsT, d_sbT)
            p1 = A.alloc([1], F32, "p1")
            k.ts("dve", p1, iof[:, 0:1], -1.0, ALU.mult, 1.0, ALU.add)
            EB = A.alloc([4, 64], F32, "EB")
            ENB = A.alloc([4, 64], F32, "ENB")
            E2 = A.alloc([4, 64], F32, "E2")
            dec = A.alloc([4], F32, "dec")
            tmpc = A.alloc([64], F32, "tmpc")
            for h in range(4):
                lg = math.log(1.0 - 2.0 ** (-5.0 - h))
                k.ts("dve", tmpc, ones[:, 0:64], p1[:, 0:1], ALU.mult)
                k.act(EB[:, h, :], tmpc, AF.Exp, scale=lg)
                k.ts("dve", EB[:, h, :], EB[:, h, :], 0.125, ALU.mult)
                k.act(ENB[:, h, :], tmpc, AF.Exp, scale=-lg)
                k.ts("dve", tmpc, tmpc, -1.0, ALU.mult, 128.0, ALU.add)
                k.act(E2[:, h, :], tmpc, AF.Exp, scale=lg)
                k.memset("dve", dec[:, h:h + 1], math.exp(lg * 128.0))
            EBv, ENBv, E2v = [t.re("p h f -> p (h f)") for t in (EB, ENB, E2)]
            Sf = A.alloc([4, 128], F32, "Sf")
            Sb = A.alloc([4, 128], BF16, "Sb")
            u = A.alloc([512], F32, "u")
            gv = A.alloc([512], F32, "gv")
            st6 = A.alloc([6], F32, "st6")
            mv = A.alloc([2], F32, "mv")
            rs1 = A.alloc([1], F32, "rs1")
            nmr = A.alloc([1], F32, "nmr")
            svn = A.alloc([512], BF16, "svn")
            qk_sb = A.alloc([512], F32, "qk_sb")
            qk_rot = A.alloc([512], F32, "qk_rot")
            rt2 = [A.alloc([256], F32, "rt%d" % i) for i in range(2)]
            rt = [rt2[0], rt2[1], rt2[0], rt2[1]]
            qkT_sb = A.alloc([8, 128], BF16, "qkT_sb")
            sT_sb = A.alloc([4, 128], BF16, "sT_sb")
            o_sb = A.alloc([512], F32, "o_sb")
            osq = A.alloc([512], F32, "osq")
            ssq = A.alloc([4], F32, "ssq")
            rs = A.alloc([4], F32, "rs")
            oT_sb = A.alloc([8, 128], BF16, "oT_sb")
            hand = [dict(q_dec=A.alloc([256], BF16, "q_dec%d" % i), k_dec=A.alloc([256], BF16, "k_dec%d" % i),
                         k2=A.alloc([256], BF16, "k2%d" % i), v_sb=A.alloc([512], BF16, "v_sb%d" % i),
                         sg=A.alloc([512], F32, "sg%d" % i), o_tok=A.alloc([1024], BF16, "o_tok%d" % i))
                    for i in range(2)]

            def odd_A(tt):
                hd = hand[tt % 2]
                q_dec, k_dec, k2, v_sb, sg, o_tok = (hd["q_dec"], hd["k_dec"], hd["k2"], hd["v_sb"],
                                                     hd["sg"], hd["o_tok"])
                h_t = hT_tile(tt)
                Z = []
                for g in range(4):
                    Z.append(PS.next())
                    for kc in range(KC):
                        k.mm(Z[g], h_t[:, kc, :], wi[:, kc, g * 512:(g + 1) * 512],
                             start=(kc == 0), stop=(kc == KC - 1))
                k.act(u, Z[0], AF.Gelu)
                k.act(gv, Z[1], AF.Gelu)
                k.copy("act", qk_sb, Z[2])
                k.copy("act", v_sb, Z[3])
                Z4 = PS.next()
                for kc in range(KC):
                    k.mm(Z4, h_t[:, kc, :], wi[:, kc, 2048:2560], start=(kc == 0), stop=(kc == KC - 1))
                k.act(sg, Z4, AF.Silu)
                S.op("dve", lambda e: e.bn_stats(out=st6.ap, in_=gv.ap), reads=[gv.buf], writes=[st6.buf])
                S.op("dve", lambda e: e.bn_aggr(out=mv.ap, in_=st6.ap), reads=[st6.buf], writes=[mv.buf])
                k.ts("dve", rs1, mv[:, 1:2], EPS, ALU.add)
                k.tt("pool", rs1, rs1, mhalf[:, 0:1], ALU.pow)
                k.stt(nmr, mv[:, 0:1], -1.0, rs1, ALU.mult, ALU.mult)
                k.act(gv, gv, AF.Identity, scale=rs1[:, 0:1], bias=nmr[:, 0:1])
                k.tt("pool", gv, gv, lngb, ALU.mult)
                k.tt("pool", svn, gv, lnbb, ALU.add)
                SG = PS.next()
                for g in range(4):
                    k.mm(SG[:, g * 128:(g + 1) * 128], wsb[:, g, :], svn[:, g * 128:(g + 1) * 128])
                for g in range(4):
                    k.stt(o_tok[:, g * 128:(g + 1) * 128], SG[:, g * 128:(g + 1) * 128], bsT[:, g:g + 1],
                          u[:, g * 128:(g + 1) * 128], ALU.add, ALU.mult)
                rope(qk_rot, qk_sb, tt, rt)
                k.tt("dve", q_dec, qk_rot[:, 0:256], EBv, ALU.mult)
                k.tt("pool", k_dec, qk_rot[:, 256:512], ENBv, ALU.mult)
                k.tt("dve", k2, qk_rot[:, 256:512], E2v, ALU.mult)

            def odd_B(tt):
                hd = hand[tt % 2]
                q_dec, k_dec, k2, v_sb, sg, o_tok = (hd["q_dec"], hd["k_dec"], hd["k2"], hd["v_sb"],
                                                     hd["sg"], hd["o_tok"])
                O = linattn_core(tt, q_dec, k_dec, k2, v_sb, dec, Sf, Sb, qkT_sb, sT_sb)
                head_rms(O, o_sb, osq, ssq, rs)
                k.tt("dve", o_sb.re("p (h f) -> p h f", h=4), o_sb.re("p (h f) -> p h f", h=4),
                     rs.bc(2, [128, 4, 128]), ALU.mult)
                k.tt("pool", o_tok[:, 512:1024], o_sb, sg, ALU.mult)
                out_proj(tt, o_tok, 8, wo, 0, gate1, oT_sb)

            interleave(lambda: odd_A(0), [0, 1, 2, 3], None, [4, 5, 6, 7])
            for tt in range(NT):
                interleave((lambda t=tt: odd_A(t + 1)) if tt + 1 < NT else None, [0, 1, 2, 3],
                           lambda t=tt: odd_B(t), [4, 5, 6, 7])
            S.barrier()
            A.release(mO)

        def moe(l):
            gate2 = mod_cols(l, 5)
            mM = A.mark()
            PS.set_rot(range(8))
            GT = A.alloc([L], F32, "GT")
            bo = A.alloc([D], F32, "bo")
            k.dma("sp", bo[0:NE, :], d_ebo[l])
            biT = A.alloc([NE, 16], F32, "biT")
            k.dma("sp", biT, d_ebi[l])
            bi1 = A.alloc([NE, 8], F32, "bi1")
            k.ts("dve", bi1, biT[:, :, 8:16], 1.0, ALU.add)
            ring = [[A.alloc([KC, 512], BF16, "wr%d_%d" % (s, i)) for i in range(2)] +
                    [A.alloc([4, D], BF16, "wr%d_2" % s)] for s in range(2)]
            wiv = [d_ewi[l, e].re("(kc p) n -> p kc n", p=128) for e in range(NE)]
            wov = [d_ewo[l, e].re("(kc p) n -> p kc n", p=128) for e in range(NE)]

            def load_half(he):
                e, hh = divmod(he, 2)
                s = he % 2
                k.dma("pool", ring[s][0], wiv[e][:, :, hh * 512:(hh + 1) * 512])
                k.dma("pool", ring[s][1], wiv[e][:, :, D + hh * 512:D + (hh + 1) * 512])
                k.dma("pool", ring[s][2], wov[e][:, hh * 4:(hh + 1) * 4, :])

            load_half(0)
            load_half(1)
            m1 = A.mark()
            rw = A.alloc([KC, NE], F32, "rw")
            k.dma("sp", rw, d_rw[l].re("(kc p) e -> p kc e", p=128))
            rb = A.alloc([NE], F32, "rb")
            k.dma("sp", rb[0:1, :], d_rb[l])
            hf = A.alloc([KC, 512], F32, "hf")
            lg4 = [A.alloc([4, NE], F32, "lg4_%d" % i) for i in range(2)]
            t84 = [A.alloc([4, 8], F32, "t84_%d" % i) for i in range(2)]
            ex4 = [A.alloc([4, NE], F32, "ex4_%d" % i) for i in range(2)]
            msk4 = [A.alloc([4, NE], F32, "msk4_%d" % i) for i in range(2)]
            sm4 = [A.alloc([4], F32, "sm4_%d" % i) for i in range(2)]
            ntm = norm_alloc()

            def router_mm(q):
                LG = PS.next()
                for r in range(4):
                    for kc in range(KC):
                        k.mm(LG[:, r * NE:(r + 1) * NE], hf[:, kc, r * 128:(r + 1) * 128], rw[:, kc, :],
                             start=(kc == 0), stop=False)
                    k.mm(LG[:, r * NE:(r + 1) * NE], ones[0:1, :], rb[0:1, :], start=False, stop=True)
                k.copy("act", lg4[q % 2], LG[:, 0:4 * NE].re("p (r e) -> p r e", r=4))

            def route_elem(q):
                lg, t8, ex, msk, sm = lg4[q % 2], t84[q % 2], ex4[q % 2], msk4[q % 2], sm4[q % 2]
                for r in range(4):
                    S.op("dve", lambda e, r=r: e.max(out=t8.ap[:, r, :], in_=lg.ap[:, r, :]),
                         reads=[lg.buf], writes=[t8.buf])
                k.tt("dve", ex, lg, V(t8.ap[:, :, 0:1].broadcast_to([128, 4, NE]), t8.buf), ALU.subtract)
                k.act(ex, ex, AF.Exp)
                k.tt("dve", msk, lg, V(t8.ap[:, :, 3:4].broadcast_to([128, 4, NE]), t8.buf), ALU.is_ge)
                k.tt("dve", ex, ex, msk, ALU.mult)
                k.red("dve", sm, ex, ALU.add)
                k.recip(sm, sm)
                k.tt("dve", ex, ex, sm.bc(2, [128, 4, NE]), ALU.mult)
                TG = PS.next()
                for r in range(4):
                    k.tr(TG[0:NE, r * 128:(r + 1) * 128], ex[:, r, :], ident)
                k.copy("act", GT[0:NE, q * 512:(q + 1) * 512], TG[0:NE, :])

            norm_quad(0, mod_cols(l, 4), mod_cols(l, 3), hT[0], out_f32=hf, nt=ntm)
            for q in range(NQ):
                router_mm(q)
                if q + 1 < NQ:
                    norm_quad(q + 1, mod_cols(l, 4), mod_cols(l, 3), hT[q + 1], out_f32=hf, nt=ntm)
                route_elem(q)
            GTb = A.alloc([L], BF16, "GTb")
            k.copy("dve", GTb[0:NE, :], GT[0:NE, :])
            k.dma("sp", d_gtb[l], GTb[0:NE, :])
            S.barrier()
            A.release(m1)
            for q in range(NQ):
                for dc in range(KC):
                    Y = PS.next()
                    k.mm(Y, bo[0:NE, dc * 128:(dc + 1) * 128], GT[0:NE, q * 512:(q + 1) * 512])
                    k.stt(xT[dc][q], Y, gate2[dc], xT[dc][q], ALU.mult, ALU.add)
            aT = [A.alloc([4, 512], BF16, "aT%d" % i) for i in range(2)]
            gate_bc = [A.alloc([L], BF16, "gate_bc%d" % i) for i in range(2)]

            def load_gate(e):
                src = d_gtb[l][e:e + 1, :]
                k.dma("sp", gate_bc[e % 2],
                      V(src.ap.partition_broadcast(128).rearrange("p o n -> p (o n)"), src.buf))
            gt = [A.alloc([512], F32, "g%d" % i) for i in range(2)]
            sgt = [A.alloc([512], F32, "s%d" % i) for i in range(2)]
            lt = [A.alloc([512], F32, "l%d" % i) for i in range(2)]
            ut = [A.alloc([512], F32, "u%d" % i) for i in range(2)]
            PS.set_rot([0, 1, 2, 3])
            ybank = [PS.fixed(4), PS.fixed(5), PS.fixed(6), PS.fixed(7)]
            load_gate(0)
            load_gate(1)
            state = {"cnt": 0, "ny": 0}

            def z_part(n, he, q):
                e, hh = divmod(he, 2)
                w_glu, w_lin, w_o = ring[he % 2]
                gs_ = gate_bc[e % 2][:, q * 512:(q + 1) * 512]
                a_ = aT[n % 2]
                pend = None
                for fc in range(4):
                    i3 = state["cnt"] % 2
                    state["cnt"] += 1
                    ZG_ = PS.next()
                    for kc in range(KC):
                        k.mm(ZG_, w_glu[:, kc, fc * 128:(fc + 1) * 128], hT[q][:, kc, :],
                             start=(kc == 0), stop=(kc == KC - 1))
                    ZL_ = PS.next()
                    for kc in range(KC):
                        k.mm(ZL_, w_lin[:, kc, fc * 128:(fc + 1) * 128], hT[q][:, kc, :],
                             start=(kc == 0), stop=(kc == KC - 1))
                    col = hh * 4 + fc
                    k.ts("dve", gt[i3], ZG_, biT[:, e, col:col + 1], ALU.add, 7.0, ALU.min)
                    k.act(sgt[i3], gt[i3], AF.Sigmoid, scale=1.702)
                    k.tt("pool", gt[i3], gt[i3], sgt[i3], ALU.mult)
                    k.act(lt[i3], ZL_, AF.Identity, bias=bi1[:, e, col:col + 1])
                    k.ts("dve", lt[i3], lt[i3], -6.0, ALU.max, 8.0, ALU.min)
                    if pend is not None:
                        pf, pi = pend
                        k.tt("dve", ut[pi], gt[pi], lt[pi], ALU.mult)
                        k.tt("pool", a_[:, pf, :], ut[pi], gs_, ALU.mult)
                    pend = (fc, i3)
                pf, pi = pend
                k.tt("dve", ut[pi], gt[pi], lt[pi], ALU.mult)
                k.tt("pool", a_[:, pf, :], ut[pi], gs_, ALU.mult)

            def y_part(n, he, q):
                w_glu, w_lin, w_o = ring[he % 2]
                a_ = aT[n % 2]
                for dc in range(KC):
                    Y = ybank[state["ny"] % 4]
                    state["ny"] += 1
                    for fc in range(4):
                        k.mm(Y, w_o[:, fc, dc * 128:(dc + 1) * 128], a_[:, fc, :],
                             start=(fc == 0), stop=(fc == 3))
                    k.stt(xT[dc][q], Y, gate2[dc], xT[dc][q], ALU.mult, ALU.add)

            steps = [(he, q) for he in range(2 * NE) for q in range(NQ)]
            for n, (he, q) in enumerate(steps):
                z_part(n, he, q)
                if n > 0:
                    y_part(n - 1, *steps[n - 1])
                if q == 1 and he >= 1 and he + 1 < 2 * NE:
                    load_half(he + 1)
                if q == 1 and he % 2 == 0 and he >= 2 and he // 2 + 1 < NE:
                    load_gate(he // 2 + 1)
            y_part(len(steps) - 1, *steps[-1])
            S.barrier()
            PS.set_rot(range(8))
            A.release(mM)

        def final_out():
            mF = A.mark()
            PS.set_rot(range(8))
            hf = A.alloc([KC, 512], F32, "hfin")
            dummy = A.alloc([KC, 512], BF16, "dummyb")
            zero = A.alloc([1], F32, "zero")
            k.memset("dve", zero, 0.0)
            ot = [A.alloc([D], F32, "ot%d" % i) for i in range(2)]
            gcols = [fgT[:, c:c + 1] for c in range(KC)]
            zcols = [zero[:, 0:1] for c in range(KC)]
            ntf = norm_alloc()
            for q in range(NQ):
                norm_quad(q, gcols, zcols, dummy, out_f32=hf, nt=ntf)
                for r in range(4):
                    tt = q * 4 + r
                    o = ot[tt % 2]
                    for half in range(2):
                        pb = PS.next()
                        for cc in range(4):
                            c = half * 4 + cc
                            k.tr(pb[:, cc * 128:(cc + 1) * 128], hf[:, c, r * 128:(r + 1) * 128], ident)
                        k.copy("act" if half else "dve", o[:, half * 512:(half + 1) * 512], pb)
                    k.dma("sp", d_out[tt * 128:(tt + 1) * 128, :], o)
            A.release(mF)

        def write_x_raw():
            mF = A.mark()
            PS.set_rot(range(8))
            ot = [A.alloc([D], F32, "ot%d" % i) for i in range(2)]
            for tt in range(NT if "nox" not in build_program.stages else 1):
                q, r = divmod(tt, 4)
                o = ot[tt % 2]
                for half in range(2):
                    pb = PS.next()
                    for cc in range(4):
                        c = half * 4 + cc
                        k.tr(pb[:, cc * 128:(cc + 1) * 128], xT[c][q][:, r * 128:(r + 1) * 128], ident)
                    k.copy("act" if half else "dve", o[:, half * 512:(half + 1) * 512], pb)
                k.dma("sp", d_out[tt * 128:(tt + 1) * 128, :], o)
            A.release(mF)

        prologue()
        for l in layers:
            if "mix" in stages:
                if l % 2 == 0:
                    even_mixer(l)
                else:
                    odd_mixer(l)
            if "moe" in stages:
                moe(l)
        if do_final:
            final_out()
        else:
            write_x_raw()
        S.barrier()
        nsem = S.emit()
        build_program.info = dict(nsem=nsem, nwaits=S.nwaits, nops=dict(S.nops), ndma=dict(S.ndma),
                                  peak_words=A.peak)
    return nc


build_program.stages = ("mix", "moe")
build_program.info = {}
build_program.dbg_tile = 0


def make_in_maps(inp, cores=range(N_CORES)):
    f = lambda a: np.ascontiguousarray(np.asarray(a))
    half = 32
    invf = (np.float32(10000.0) ** (-(np.arange(half, dtype=np.float32) / np.float32(half)))).astype(np.float32)
    shared = {
        "invf": f(np.broadcast_to(invf[None, :], (128, half))),
        "w_ada": f(inp["w_ada"]),
        "b_adaT": f(np.asarray(inp["b_ada"]).reshape(2, 48, 128).transpose(2, 0, 1)),
        "even_w_in": f(inp["even_w_in"][0]),
        "gla_w_gate": f(inp["gla_w_gate"][0]),
        "gla_b_gate": f(inp["gla_b_gate"]),
        "gla_norm_g": f(inp["gla_norm_g"]),
        "lam_in": f(np.concatenate([inp["diff_lam_q1"][0], inp["diff_lam_q2"][0],
                                    inp["diff_lam_k1"][0], inp["diff_lam_k2"][0]])[None, :]),
        "diff_norm_g": f(inp["diff_norm_g"]),
        "even_w_out": f(inp["even_w_out"][0]),
        "odd_w_in": f(inp["odd_w_in"][0]),
        "sgu_ln_g": f(inp["sgu_ln_g"]),
        "sgu_ln_b": f(inp["sgu_ln_b"]),
        "sgu_wT": f(np.asarray(inp["sgu_w"][0]).transpose(0, 2, 1)),
        "sgu_bT": f(np.asarray(inp["sgu_b"][0]).T),
        "odd_w_out": f(inp["odd_w_out"][0]),
        "router_w": f(inp["router_w"]),
        "router_b": f(np.asarray(inp["router_b"])[:, None, :]),
        "expert_w_in": f(inp["expert_w_in"]),
        "expert_b_inT": f(np.asarray(inp["expert_b_in"]).reshape(2, NE, 16, 128).transpose(0, 3, 1, 2)),
        "expert_w_out": f(inp["expert_w_out"]),
        "expert_b_out": f(inp["expert_b_out"]),
        "final_gT": f(np.asarray(inp["final_norm_g"]).reshape(KC, 128).T),
    }
    maps = []
    for b in cores:
        m = dict(shared)
        m["x"] = f(inp["x"][b])
        m["cT"] = f(np.asarray(inp["c"][b]).reshape(KC, 128).T)
        m["posT"] = f(np.asarray(inp["positions"][b]).astype(np.int32).reshape(NT, 128).T)
        maps.append({n: m[n] for n in build_program.in_names})
    return maps


def kernel(**inputs):
    build_program.stages = ("mix", "moe")
    nc = build_program(layers=(0, 1), do_final=True)
    maps = make_in_maps(inputs)
    res = run_bass_kernel_spmd(nc, maps, core_ids=list(range(N_CORES)))
    return np.stack([np.asarray(r["yout"]) for r in res.results], axis=0).astype(np.float32)
```

```python
from contextlib import ExitStack
import math
import threading
import numpy as np
import concourse.bass as bass
import concourse.mybir as mybir
from concourse.bass_utils import run_bass_kernel_spmd

F32 = mybir.dt.float32
BF16 = mybir.dt.bfloat16
I32 = mybir.dt.int32
AF = mybir.ActivationFunctionType
ALU = mybir.AluOpType
AX = mybir.AxisListType

D = 1024
L = 2048
NT = 16
NQ = 4
KC = 8
NE = 32
EPS = 1e-6
N_CORES = 8


class Buf:
    __slots__ = ("name", "w", "r", "excl")

    def __init__(self, name="", excl=False):
        self.name = name
        self.w = None
        self.r = {}
        self.excl = excl


class Sched:
    ENGS = ("pe", "act", "dve", "pool", "sp")
    LIMIT = 30000
    NSLOT = 8

    def __init__(self, nc, stack):
        self.nc = nc
        self.stack = stack
        self.eng = {"pe": nc.tensor, "act": nc.scalar, "dve": nc.vector,
                    "pool": nc.gpsimd, "sp": nc.sync}
        self.ops = []
        self.nops = {e: 0 for e in self.ENGS}
        self.seen = {e: {} for e in self.ENGS}
        self.ndma = {e: 0 for e in self.ENGS}
        self.marked = set()
        self.last_dma = {}
        self.il = None

    def _deps(self, eng, reads, writes, skip_same):
        deps = {}

        def add(tok):
            if tok is None:
                return
            k, i = tok
            if skip_same and k == eng:
                return
            if deps.get(k, -1) < i:
                deps[k] = i
        for b in reads:
            add(b.w)
        for b in writes:
            add(b.w)
            for k, i in b.r.items():
                add((k, i))
        seen = self.seen[eng]
        waits = []
        for k, i in deps.items():
            if seen.get(k, -1) >= i:
                continue
            seen[k] = i
            waits.append((k, i))
            self.marked.add((k, i))
        return waits

    def _update(self, tok, reads, writes):
        k, i = tok
        for b in reads:
            if b.r.get(k, -1) < i:
                b.r[k] = i
        for b in writes:
            b.w = tok
            b.r = {}

    def op(self, eng, fn, reads=(), writes=()):
        il = self.il
        if il is not None:
            il.acquire()
        try:
            return self._op(eng, fn, reads, writes)
        finally:
            if il is not None:
                il.release()

    def _op(self, eng, fn, reads=(), writes=()):
        ex = [b for b in reads if b.excl]
        if ex:
            writes = list(writes) + [b for b in ex if b not in writes]
            reads = [b for b in reads if not b.excl]
        waits = self._deps(eng, reads, writes, eng == "pe")
        idx = self.nops[eng]
        self.nops[eng] += 1
        tok = (eng, idx)
        self.ops.append((eng, fn, waits, "c", eng, idx))
        self._update(tok, reads, writes)
        return tok

    def dma(self, eng, fn, reads=(), writes=()):
        assert self.il is None, "no DMA inside interleaved sections"
        n = self.ndma[eng]
        self.ndma[eng] += 1
        slot = n % self.NSLOT
        key = ("d", eng, slot)
        idx = n // self.NSLOT
        waits = self._deps(eng, reads, writes, False)
        if idx > 0:
            seen = self.seen[eng]
            if seen.get(key, -1) < idx - 1:
                seen[key] = idx - 1
                waits.append((key, idx - 1))
        tok = (key, idx)
        self.last_dma[key] = idx
        self.ops.append((eng, fn, waits, "d", key, idx))
        self._update(tok, reads, writes)
        return tok

    def barrier(self):
        if "nobar" in build_program.stages:
            return
        toks = []
        for e in self.ENGS:
            if self.nops[e] > 0:
                toks.append((e, self.nops[e] - 1))
        for key, idx in self.last_dma.items():
            toks.append((key, idx))
        for e in self.ENGS:
            seen = self.seen[e]
            waits = []
            for (k, i) in toks:
                if seen.get(k, -1) >= i:
                    continue
                seen[k] = i
                waits.append((k, i))
                self.marked.add((k, i))
            if waits:
                self.ops.append((e, None, waits, "w", None, None))

    def wait_all(self, eng, bufs):
        waits = self._deps(eng, bufs, (), False)
        self.ops.append((eng, None, waits, "w", None, None))

    def emit(self):
        nc = self.nc
        sems = {}

        def get_sem(name):
            if name not in sems:
                sems[name] = self.stack.enter_context(nc.semaphore(name))
            return sems[name]

        val = {}
        counters = {e: 0 for e in self.ENGS}
        for (eng, fn, waits, kind, key, idx) in self.ops:
            if kind == "c" and (key, idx) in self.marked:
                c = counters[eng]
                counters[eng] += 1
                val[(key, idx)] = ("s_%s_%d" % (eng, c // self.LIMIT), (c % self.LIMIT) + 1)
        nw = 0
        for (eng, fn, waits, kind, key, idx) in self.ops:
            e = self.eng[eng]
            for (k, i) in waits:
                if isinstance(k, tuple):
                    sname = "d_%s_%d" % (k[1], k[2])
                    v = 16 * (i + 1)
                else:
                    sname, v = val[(k, i)]
                e.wait_ge(get_sem(sname), v)
                nw += 1
            if fn is None:
                continue
            ins = fn(e)
            if kind == "d":
                ins.then_inc(get_sem("d_%s_%d" % (key[1], key[2])), 16)
            elif (key, idx) in self.marked:
                ins.then_inc(get_sem(val[(key, idx)][0]), 1)
        self.nwaits = nw
        return len(sems)


class Interleaver:
    def __init__(self):
        self.cv = threading.Condition()
        self.turn = 0
        self.alive = [True, True]
        self.ids = {}

    def register(self, i):
        self.ids[threading.get_ident()] = i

    def acquire(self):
        me = self.ids[threading.get_ident()]
        with self.cv:
            while self.turn != me:
                self.cv.wait()

    def release(self):
        me = self.ids[threading.get_ident()]
        with self.cv:
            if self.alive[1 - me]:
                self.turn = 1 - me
            self.cv.notify_all()

    def finish(self, i):
        with self.cv:
            self.alive[i] = False
            self.turn = 1 - i
            self.cv.notify_all()


class V:
    __slots__ = ("ap", "buf")

    def __init__(self, ap, buf=None, name=""):
        self.ap = ap
        self.buf = buf if buf is not None else Buf(name)

    def __getitem__(self, k):
        return V(self.ap[k], self.buf)

    def bitcast(self, dt):
        return V(self.ap.bitcast(dt), self.buf)

    def re(self, s, **kw):
        return V(self.ap.rearrange(s, **kw), self.buf)

    def bc(self, axis, shape):
        return V(self.ap.unsqueeze(axis).broadcast_to(shape), self.buf)


def _a(x):
    return x.ap if isinstance(x, V) else x


def _bufs(*xs):
    out = []
    for x in xs:
        if isinstance(x, V) and x.buf not in out:
            out.append(x.buf)
    return out


class K:
    def __init__(self, S):
        self.S = S

    def tt(self, eng, out, a, b, op):
        return self.S.op(eng, lambda e: e.tensor_tensor(out=out.ap, in0=a.ap, in1=b.ap, op=op),
                         reads=_bufs(a, b), writes=_bufs(out))

    def ts(self, eng, out, a, s1, op0, s2=None, op1=None):
        kw = {}
        if op1 is not None:
            kw["op1"] = op1
        return self.S.op(eng, lambda e: e.tensor_scalar(out=out.ap, in0=a.ap, scalar1=_a(s1), scalar2=_a(s2),
                                                        op0=op0, **kw),
                         reads=_bufs(a, s1, s2), writes=_bufs(out))

    def stt(self, out, a, s, b, op0, op1):
        return self.S.op("dve", lambda e: e.scalar_tensor_tensor(out=out.ap, in0=a.ap, scalar=_a(s), in1=b.ap,
                                                                 op0=op0, op1=op1),
                         reads=_bufs(a, s, b), writes=_bufs(out))

    def act(self, out, a, func, scale=1.0, bias=None, accum=None):
        kw = {}
        if bias is not None:
            kw["bias"] = _a(bias)
        if accum is not None:
            kw["accum_out"] = accum.ap
        return self.S.op("act", lambda e: e.activation(out=out.ap, in_=a.ap, func=func, scale=_a(scale), **kw),
                         reads=_bufs(a, scale, bias), writes=_bufs(out, accum))

    def copy(self, eng, out, a):
        if eng == "act":
            return self.S.op(eng, lambda e: e.copy(out=out.ap, in_=a.ap), reads=_bufs(a), writes=_bufs(out))
        return self.S.op(eng, lambda e: e.tensor_copy(out=out.ap, in_=a.ap), reads=_bufs(a), writes=_bufs(out))

    def memset(self, eng, out, val):
        return self.S.op(eng, lambda e: e.memset(out.ap, val), writes=_bufs(out))

    def mm(self, out, lhsT, rhs, start=True, stop=True):
        return self.S.op("pe", lambda e: e.matmul(out.ap, lhsT=lhsT.ap, rhs=rhs.ap, start=start, stop=stop),
                         reads=_bufs(lhsT, rhs), writes=_bufs(out))

    def tr(self, out, a, ident):
        return self.S.op("pe", lambda e: e.transpose(out.ap, a.ap, ident.ap),
                         reads=_bufs(a, ident), writes=_bufs(out))

    def red(self, eng, out, a, op, axis=AX.X):
        return self.S.op(eng, lambda e: e.tensor_reduce(out=out.ap, in_=a.ap, axis=axis, op=op),
                         reads=_bufs(a), writes=_bufs(out))

    def recip(self, out, a):
        return self.S.op("dve", lambda e: e.reciprocal(out=out.ap, in_=a.ap), reads=_bufs(a), writes=_bufs(out))

    def dma(self, eng, out, a, **kw):
        return self.S.dma(eng, lambda e: e.dma_start(out=out.ap, in_=a.ap, **kw),
                          reads=_bufs(a), writes=_bufs(out))


class Arena:
    def __init__(self, ap_f32, nwords):
        self.base = ap_f32
        self.n = nwords
        self.off = 0
        self.peak = 0

    def mark(self):
        return self.off

    def release(self, m):
        self.off = m

    def alloc(self, shape, dtype=F32, name="", parts=128):
        n = int(np.prod(shape))
        esz = 4 if dtype in (F32, I32) else 2
        words = (n * esz + 3) // 4
        assert self.off + words <= self.n, ("SBUF arena overflow", name, self.off, words, self.n)
        ap = self.base[0:parts, self.off:self.off + words]
        self.off += words
        self.peak = max(self.peak, self.off)
        if dtype != F32:
            ap = ap.bitcast(dtype)
        ap = ap[:, 0:n]
        if len(shape) > 1:
            names = " ".join("d%d" % i for i in range(len(shape)))
            kw = {"d%d" % i: int(s) for i, s in enumerate(shape)}
            ap = ap.rearrange("p (%s) -> p %s" % (names, names), **kw)
        return V(ap, Buf(name))


class PsPool:
    def __init__(self, banks):
        self.banks = banks
        self.rot = list(range(len(banks)))
        self.i = 0
        self.tl = {}

    def set_rot(self, idxs):
        self.rot = list(idxs)
        self.i = 0

    def next(self):
        st = self.tl.get(threading.get_ident())
        if st is not None:
            b = self.banks[st[0][st[1] % len(st[0])]]
            st[1] += 1
            return b
        b = self.banks[self.rot[self.i % len(self.rot)]]
        self.i += 1
        return b

    def fixed(self, i):
        return self.banks[i]


ARENA_WORDS = 52800


def build_program(layers=(0, 1), do_final=True, dbg=None):
    nc = bass.Bass("TRN2", target_bir_lowering=False)

    stages = build_program.stages
    in_names = []

    def din(name, shape, dt=F32, used=True):
        if not used:
            return None
        in_names.append(name)
        return V(nc.dram_tensor(name, list(shape), dt, kind="ExternalInput").ap(), name=name)

    has_mix0 = (0 in layers) and ("mix" in stages)
    has_mix1 = (1 in layers) and ("mix" in stages)
    has_moe = len(layers) > 0 and ("moe" in stages)
    has_mod = len(layers) > 0 and ("nomod" not in stages)
    d_x = din("x", [L, D])
    d_cT = din("cT", [128, KC])
    d_pos = din("posT", [128, NT], I32)
    d_invf = din("invf", [128, 32])
    d_wada = din("w_ada", [2, D, 6 * D], used=has_mod)
    d_bada = din("b_adaT", [128, 2, 48])
    d_ewin = din("even_w_in", [D, 3088], used=has_mix0)
    d_wg = din("gla_w_gate", [16, 256], used=has_mix0)
    d_bg = din("gla_b_gate", [1, 256], used=has_mix0)
    d_glag = din("gla_norm_g", [1, 128], used=has_mix0)
    d_lam = din("lam_in", [1, 256], used=has_mix0)
    d_diffg = din("diff_norm_g", [1, 128], used=has_mix0)
    d_ewout = din("even_w_out", [D, D], used=has_mix0)
    d_owin = din("odd_w_in", [D, 2560], used=has_mix1)
    d_lng = din("sgu_ln_g", [1, 512], used=has_mix1)
    d_lnb = din("sgu_ln_b", [1, 512], used=has_mix1)
    d_swT = din("sgu_wT", [4, 128, 128], used=has_mix1)
    d_sbT = din("sgu_bT", [128, 4], used=has_mix1)
    d_owout = din("odd_w_out", [D, D], used=has_mix1)
    d_rw = din("router_w", [2, D, NE], used=has_moe)
    d_rb = din("router_b", [2, 1, NE], used=has_moe)
    d_ewi = din("expert_w_in", [2, NE, D, 2 * D], used=has_moe)
    d_ebi = din("expert_b_inT", [2, 128, NE, 16], used=has_moe)
    d_ewo = din("expert_w_out", [2, NE, D, D], used=has_moe)
    d_ebo = din("expert_b_out", [2, NE, D], used=has_moe)
    d_fg = din("final_gT", [128, KC])
    build_program.in_names = in_names
    d_out = V(nc.dram_tensor("yout", [L, D], F32, kind="ExternalOutput").ap(), name="out")
    d_gtb = [V(nc.dram_tensor("gtb%d" % l, [NE, L], BF16, kind="Internal").ap(), name="gtb%d" % l) for l in range(2)]
    d_dbg = None
    if dbg is not None:
        d_dbg = V(nc.dram_tensor("dbg", [128, dbg], F32, kind="ExternalOutput").ap(), name="dbg")

    with ExitStack() as st:
        S = Sched(nc, st)
        k = K(S)
        big = st.enter_context(nc.sbuf_tensor("arena", [128, ARENA_WORDS], F32))
        A = Arena(big, ARENA_WORDS)
        banks = [V(st.enter_context(nc.psum_tensor("ps%d" % i, [128, 512], F32))[:, :], Buf("ps%d" % i, excl=True))
                 for i in range(8)]
        PS = PsPool(banks)

        def interleave(fA, rotA, fB, rotB):
            il = Interleaver()
            errs = []

            def wrap(i, f, rot):
                il.register(i)
                PS.tl[threading.get_ident()] = [list(rot), 0]
                try:
                    if f is not None:
                        f()
                except BaseException as ex:
                    errs.append(ex)
                finally:
                    PS.tl.pop(threading.get_ident(), None)
                    il.finish(i)
            S.il = il
            ts = [threading.Thread(target=wrap, args=(0, fA, rotA)),
                  threading.Thread(target=wrap, args=(1, fB, rotB))]
            for t in ts:
                t.start()
            for t in ts:
                t.join()
            S.il = None
            if errs:
                raise errs[0]

        xT_all = A.alloc([KC, L], F32, "xT")
        xT = [[V(xT_all.ap[:, c, q * 512:(q + 1) * 512], Buf("xT%d_%d" % (c, q))) for q in range(NQ)]
              for c in range(KC)]
        hT_all = A.alloc([KC, L], BF16, "hT")
        hT = [V(hT_all.ap[:, :, q * 512:(q + 1) * 512], Buf("hT%d" % q)) for q in range(NQ)]

        def hT_tile(tt):
            q, r = divmod(tt, 4)
            return hT[q][:, :, r * 128:(r + 1) * 128]

        ident = A.alloc([128], F32, "ident")
        identb = A.alloc([128], BF16, "identb")
        tri = A.alloc([128], F32, "tri")
        trib = A.alloc([128], BF16, "trib")
        tri16 = A.alloc([128], F32, "tri16")
        ones = A.alloc([128], F32, "ones")
        ones16 = A.alloc([128], F32, "ones16")
        mhalf = A.alloc([8], F32, "mhalf")
        mod = A.alloc([2, 48], F32, "mod")
        fgT = A.alloc([KC], F32, "fgT")
        cosT = A.alloc([NT, 32], F32, "cos")
        sinT = A.alloc([NT, 32], F32, "sin")

        io = A.alloc([128], I32, "io")
        S.op("pool", lambda e: e.iota(io.ap, pattern=[[1, 128]], base=0, channel_multiplier=-1),
             writes=[io.buf])
        iof = A.alloc([128], F32, "iof")
        k.copy("dve", iof, io)
        k.ts("dve", tri, iof, 0.0, ALU.is_ge)
        k.ts("dve", ident, iof, 0.0, ALU.is_equal)
        k.copy("dve", identb, ident)
        k.copy("dve", trib, tri)
        k.ts("dve", tri16, tri, 1.0 / 16.0, ALU.mult)
        k.memset("dve", ones, 1.0)
        k.memset("dve", ones16, 1.0 / 16.0)
        k.memset("dve", mhalf, -0.5)
        k.dma("sp", fgT, d_fg)

        dbg_off = [0]

        def dump(v, ncols, parts=128):
            if d_dbg is None:
                return
            m = A.mark()
            t = A.alloc([ncols], F32, "dbgt")
            k.copy("dve", t[0:parts, :], v)
            k.dma("sp", d_dbg[0:parts, dbg_off[0]:dbg_off[0] + ncols], t[0:parts, :])
            S.barrier()
            A.release(m)
            dbg_off[0] += ncols

        def prologue():
            m0 = A.mark()
            PS.set_rot(range(8))
            cT = A.alloc([KC], F32, "cT")
            k.dma("sp", cT, d_cT)
            cact = A.alloc([KC], F32, "cact")
            k.act(cact, cT, AF.Silu)
            badaT = A.alloc([2, 48], F32, "badaT")
            k.dma("sp", badaT, d_bada)
            wa = [A.alloc([KC, 512], F32, "wa%d" % i) for i in range(2)]
            nld = 0
            for l in range(2):
                if l not in layers or "nomod" in build_program.stages:
                    continue
                pm = PS.next()
                wv = d_wada[l].re("(kc p) n -> p kc n", p=128)
                for g in range(12):
                    w = wa[nld % 2]
                    nld += 1
                    k.dma("sp", w, wv[:, :, g * 512:(g + 1) * 512])
                    for jj in range(4):
                        j = g * 4 + jj
                        for kc in range(KC):
                            k.mm(pm[:, j:j + 1], w[:, kc, jj * 128:(jj + 1) * 128], cact[:, kc:kc + 1],
                                 start=(kc == 0), stop=(kc == KC - 1))
                k.tt("dve", mod[:, l, :], pm[:, 0:48], badaT[:, l, :], ALU.add)
                k.ts("dve", mod[:, l, 8:16], mod[:, l, 8:16], 1.0, ALU.add)
                k.ts("dve", mod[:, l, 32:40], mod[:, l, 32:40], 1.0, ALU.add)
            xin = [A.alloc([D], F32, "xin%d" % i) for i in range(2)]
            for tt in range(NT if "nox" not in build_program.stages else 1):
                xi = xin[tt % 2]
                k.dma("sp", xi, d_x[tt * 128:(tt + 1) * 128, :])
                q, r = divmod(tt, 4)
                for half in range(2):
                    pb = PS.next()
                    for cc in range(4):
                        c = half * 4 + cc
                        k.tr(pb[:, cc * 128:(cc + 1) * 128], xi[:, c * 128:(c + 1) * 128], ident)
                    for cc in range(4):
                        c = half * 4 + cc
                        k.copy("act" if cc % 2 else "dve", xT[c][q][:, r * 128:(r + 1) * 128],
                               pb[:, cc * 128:(cc + 1) * 128])
            posi = A.alloc([NT], I32, "posi")
            k.dma("sp", posi, d_pos)
            posf = A.alloc([NT], F32, "posf")
            k.copy("dve", posf, posi)
            invf = A.alloc([32], F32, "invf")
            k.dma("sp", invf, d_invf)
            ang = A.alloc([NT, 32], F32, "ang")
            if "noang" not in build_program.stages:
                k.tt("dve", ang, posf.bc(2, [128, NT, 32]), invf.bc(1, [128, NT, 32]), ALU.mult)
            C1 = 6.28125
            C2 = 2.0 * math.pi - C1
            TWO_PI = 2.0 * math.pi

            def reduce_sin(dst, src_ang, shift):
                a = A.alloc([NT, 32], F32, "rs_a")
                if shift != 0.0:
                    k.ts("dve", a, src_ang, shift, ALU.add)
                else:
                    k.copy("dve", a, src_ang)
                nf = A.alloc([NT, 32], F32, "rs_n")
                k.ts("dve", nf, a, 1.0 / TWO_PI, ALU.mult)
                ni = A.alloc([NT, 32], I32, "rs_ni")
                k.copy("dve", ni, nf)
                k.copy("dve", nf, ni)
                r = A.alloc([NT, 32], F32, "rs_r")
                k.stt(r, nf, -C1, a, ALU.mult, ALU.add)
                k.stt(r, nf, -C2, r, ALU.mult, ALU.add)
                m1 = A.alloc([NT, 32], F32, "rs_m")
                k.ts("dve", m1, r, math.pi, ALU.is_gt)
                k.stt(r, m1, -TWO_PI, r, ALU.mult, ALU.add)
                k.ts("dve", m1, r, -math.pi, ALU.is_lt)
                k.stt(r, m1, TWO_PI, r, ALU.mult, ALU.add)
                k.ts("dve", r, r, math.pi, ALU.min, -math.pi, ALU.max)
                k.act(dst, r, AF.Sin)

            if "norope" not in build_program.stages:
                reduce_sin(sinT, ang, 0.0)
                reduce_sin(cosT, ang, math.pi / 2.0)
            S.barrier()
            A.release(m0)

        def norm_alloc():
            return dict(sq=[A.alloc([512], F32, "sq%d" % i) for i in range(2)],
                        sd=A.alloc([512], F32, "sd"), rstd=A.alloc([512], F32, "rstd"),
                        tmp=[A.alloc([512], F32, "ntmp%d" % i) for i in range(2)])

        def norm_quad(q, scale_cols, shift_cols, out_bf, out_f32=None, nt=None):
            sq, sd, rstd, tmp = nt["sq"], nt["sd"], nt["rstd"], nt["tmp"]
            pss = PS.next()
            for c in range(KC):
                k.act(sq[c % 2], xT[c][q], AF.Square)
                k.mm(pss, ones, sq[c % 2], start=(c == 0), stop=(c == KC - 1))
            k.act(sd, pss, AF.Sqrt, scale=1.0 / D, bias=EPS)
            k.recip(rstd, sd)
            for c in range(KC):
                t = tmp[c % 2]
                k.tt("dve" if c % 2 else "pool", t, xT[c][q], rstd, ALU.mult)
                if out_f32 is not None:
                    k.act(out_f32[:, c, :], t, AF.Identity, scale=scale_cols[c], bias=shift_cols[c])
                    k.copy("pool", out_bf[:, c, :], out_f32[:, c, :])
                else:
                    k.act(out_bf[:, c, :], t, AF.Identity, scale=scale_cols[c], bias=shift_cols[c])

        def norm_all(scale_cols, shift_cols):
            mN = A.mark()
            nt = norm_alloc()
            for q in range(NQ):
                norm_quad(q, scale_cols, shift_cols, hT[q], nt=nt)
            S.barrier()
            A.release(mN)

        def mod_cols(l, j):
            return [mod[:, l, j * 8 + c: j * 8 + c + 1] for c in range(KC)]

        def rope(dst, src, tt, tmp4):
            sv = src.re("p (g two f) -> p g two f", g=8, two=2)
            dv = dst.re("p (g two f) -> p g two f", g=8, two=2)
            x1, x2 = sv[:, :, 0, :], sv[:, :, 1, :]
            cs = cosT[:, tt, :].bc(1, [128, 8, 32])
            sn = sinT[:, tt, :].bc(1, [128, 8, 32])
            t1, t2, t3, t4 = [t.re("p (g f) -> p g f", g=8) for t in tmp4]
            k.tt("dve", t1, x1, cs, ALU.mult)
            k.tt("pool", t2, x2, sn, ALU.mult)
            k.tt("dve", dv[:, :, 0, :], t1, t2, ALU.subtract)
            k.tt("pool", t3, x2, cs, ALU.mult)
            k.tt("dve", t4, x1, sn, ALU.mult)
            k.tt("pool", dv[:, :, 1, :], t3, t4, ALU.add)

        def linattn_core(tt, q_dec, k_dec, k2, v_sb, dec, Sf, Sb, qkT_sb, sT_sb):
            QT = PS.next()
            QTb = QT.bitcast(BF16).re("p (g f) -> p g f", g=8)
            for h in range(4):
                k.tr(QTb[0:64, h, :], q_dec[:, h * 64:(h + 1) * 64], identb)
                k.tr(QTb[0:64, 4 + h, :], k_dec[:, h * 64:(h + 1) * 64], identb)
            k.copy("act", qkT_sb[0:64], QTb[0:64])
            ST = PS.next()
            for h in range(4):
                k.mm(ST[:, h * 128:(h + 1) * 128], qkT_sb[0:64, 4 + h, :], qkT_sb[0:64, h, :])
            k.tt("dve", sT_sb, ST.re("p (h f) -> p h f", h=4), tri.bc(1, [128, 4, 128]), ALU.mult)
            O = PS.next()
            for h in range(4):
                k.mm(O[:, h * 128:(h + 1) * 128], sT_sb[:, h, :], v_sb[:, h * 128:(h + 1) * 128],
                     start=True, stop=(tt == 0))
                if tt > 0:
                    k.mm(O[:, h * 128:(h + 1) * 128], qkT_sb[0:64, h, :], Sb[0:64, h, :],
                         start=False, stop=True)
            if tt < NT - 1:
                KV = PS.next()
                for h in range(4):
                    k.mm(KV[0:64, h * 128:(h + 1) * 128], k2[:, h * 64:(h + 1) * 64],
                         v_sb[:, h * 128:(h + 1) * 128])
                for h in range(4):
                    if tt == 0:
                        k.copy("dve", Sf[0:64, h, :], KV[0:64, h * 128:(h + 1) * 128])
                    else:
                        k.stt(Sf[0:64, h, :], Sf[0:64, h, :], dec[0:64, h:h + 1],
                              KV[0:64, h * 128:(h + 1) * 128], ALU.mult, ALU.add)
                k.copy("act", Sb[0:64], Sf[0:64])
            return O

        def head_rms(O, o_sb, osq, ssq, rs):
            k.copy("act", o_sb, O)
            k.tt("pool", osq, o_sb, o_sb, ALU.mult)
            k.red("dve", ssq, osq.re("p (h f) -> p h f", h=4), ALU.add)
            k.ts("dve", ssq, ssq, 1.0 / 128.0, ALU.mult, EPS, ALU.add)
            k.tt("pool", rs, ssq, mhalf[:, 0:4], ALU.pow)

        def out_proj(tt, o_tok, nk, wo_sb, kc0, gate_cols, oT_sb, src_bufs=None):
            q, r = divmod(tt, 4)
            OT = PS.next()
            OTb = OT.bitcast(BF16)
            for kc in range(nk):
                src = o_tok[:, kc * 128:(kc + 1) * 128]
                if src_bufs is not None:
                    src = V(src.ap, src_bufs[kc].buf)
                k.tr(OTb[:, kc * 128:(kc + 1) * 128], src, identb)
            k.copy("act", oT_sb[:, 0:nk, :], OTb[:, 0:nk * 128].re("p (g f) -> p g f", g=nk))
            for half in range(2):
                Y = PS.next()
                for dd in range(4):
                    dc = half * 4 + dd
                    for kc in range(nk):
                        k.mm(Y[:, dd * 128:(dd + 1) * 128], wo_sb[:, kc0 + kc, dc * 128:(dc + 1) * 128],
                             oT_sb[:, kc, :], start=(kc == 0), stop=(kc == nk - 1))
                for dd in range(4):
                    dc = half * 4 + dd
                    xs = xT[dc][q][:, r * 128:(r + 1) * 128]
                    k.stt(xs, Y[:, dd * 128:(dd + 1) * 128], gate_cols[dc], xs, ALU.mult, ALU.add)

        def load_bc_row(dst, dsrc, n):
            k.dma("sp", dst, V(dsrc.ap.partition_broadcast(128).rearrange("p o n -> p (o n)"), dsrc.buf))

        def even_mixer(l):
            gate1 = mod_cols(l, 2)
            mE = A.mark()
            PS.set_rot(range(8))
            norm_all(mod_cols(l, 1), mod_cols(l, 0))
            wdv = d_ewin.re("(kc p) n -> p kc n", p=128)
            mB = A.mark()
            NTB = 0 if "nogla" in build_program.stages else NT
            wa_ = A.alloc([KC, 1552], BF16, "wga")
            for (c0, c1) in ((0, 512), (512, 1024), (1024, 1552)):
                k.dma("pool", wa_[:, :, c0:c1], wdv[:, :, c0:c1])
            wo = A.alloc([4, D], BF16, "wo")
            k.dma("pool", wo, d_ewout.re("(kc p) n -> p kc n", p=128)[:, 0:4, :])
            wg = A.alloc([256], F32, "wg")
            k.dma("sp", wg[0:16, :], d_wg)
            bg = A.alloc([256], F32, "bg")
            k.dma("sp", bg[0:1, :], d_bg)
            ggb = A.alloc([128], F32, "ggb")
            load_bc_row(ggb, d_glag, 128)
            Sf = A.alloc([4, 128], F32, "Sf")
            Sb = A.alloc([4, 128], BF16, "Sb")
            grT = A.alloc([128], F32, "grT")
            ax = A.alloc([256], F32, "ax")
            ln_ = A.alloc([256], F32, "ln")
            la = A.alloc([256], F32, "la")
            b_sb = A.alloc([256], F32, "b_sb")
            eb = A.alloc([256], F32, "eb")
            enb = A.alloc([256], F32, "enb")
            e2 = A.alloc([256], F32, "e2")
            dec = A.alloc([4], F32, "dec")
            q_dec = A.alloc([256], BF16, "q_dec")
            k_dec = A.alloc([256], BF16, "k_dec")
            k2 = A.alloc([256], BF16, "k2")
            v_sb = A.alloc([512], BF16, "v_sb")
            qkT_sb = A.alloc([8, 128], BF16, "qkT_sb")
            sT_sb = A.alloc([4, 128], BF16, "sT_sb")
            o_sb = A.alloc([512], F32, "o_sb")
            osq = A.alloc([512], F32, "osq")
            ssq = A.alloc([4], F32, "ssq")
            rs = A.alloc([4], F32, "rs")
            sg = A.alloc([512], F32, "sg")
            o_tok = A.alloc([512], BF16, "o_tok")
            oT_sb = A.alloc([8, 128], BF16, "oT_sb")
            hand = [dict(q_dec=A.alloc([256], BF16, "q_dec%d" % i), k_dec=A.alloc([256], BF16, "k_dec%d" % i),
                         k2=A.alloc([256], BF16, "k2%d" % i), v_sb=A.alloc([512], BF16, "v_sb%d" % i),
                         dec=A.alloc([4], F32, "dec%d" % i), sg=A.alloc([512], F32, "sg%d" % i)) for i in range(2)]

            def gla_A(tt):
                hd = hand[tt % 2]
                q_dec, k_dec, k2, v_sb, dec, sg = hd["q_dec"], hd["k_dec"], hd["k2"], hd["v_sb"], hd["dec"], hd["sg"]
                h_t = hT_tile(tt)
                ZA = PS.fixed(0)
                ZB, ZG, ZR = PS.next(), PS.next(), PS.next()
                for (Zp, c0) in ((ZA, 0), (ZB, 512), (ZG, 1040)):
                    for kc in range(KC):
                        k.mm(Zp, h_t[:, kc, :], wa_[:, kc, c0:c0 + 512], start=(kc == 0), stop=(kc == KC - 1))
                for kc in range(KC):
                    k.mm(ZR[0:16, 0:128], wa_[:, kc, 1024:1040], h_t[:, kc, :],
                         start=(kc == 0), stop=(kc == KC - 1))
                k.copy("act", grT[0:16, :], ZR[0:16, 0:128])
                k.act(sg, ZG, AF.Silu)
                k.copy("act", v_sb, ZB)
                GP = PS.next()
                k.mm(GP[:, 0:256], grT[0:16, :], wg[0:16, :], start=True, stop=False)
                k.mm(GP[:, 0:256], ones[0:1, :], bg[0:1, :], start=False, stop=True)
                k.act(ax, GP[:, 0:256], AF.Abs)
                k.act(ax, ax, AF.Exp, scale=-1.0)
                k.act(ln_, ax, AF.Ln, bias=1.0)
                k.ts("dve", la, GP[:, 0:256], 0.0, ALU.min)
                k.tt("dve", la, la, ln_, ALU.subtract)
                B2 = PS.next()
                k.mm(B2[:, 0:256], tri16, la)
                k.mm(B2[:, 256:512], ones16, la)
                DC = PS.next()
                for h in range(4):
                    k.mm(DC[0:64, h:h + 1], la[:, h * 64:(h + 1) * 64], ones16[:, 0:1])
                k.copy("dve", b_sb, B2[:, 0:256])
                k.act(eb, B2[:, 0:256], AF.Exp)
                k.act(enb, B2[:, 0:256], AF.Exp, scale=-1.0)
                k.tt("dve", e2, B2[:, 256:512], b_sb, ALU.subtract)
                k.act(e2, e2, AF.Exp)
                k.act(dec[0:64, :], DC[0:64, 0:4], AF.Exp)
                k.stt(q_dec, ZA[:, 0:256], 0.125, eb, ALU.mult, ALU.mult)
                k.tt("dve", k_dec, ZA[:, 256:512], enb, ALU.mult)
                k.tt("dve", k2, ZA[:, 256:512], e2, ALU.mult)
                k.tt("pool", sg.re("p (h f) -> p h f", h=4), sg.re("p (h f) -> p h f", h=4),
                     ggb.bc(1, [128, 4, 128]), ALU.mult)

            def gla_B(tt):
                hd = hand[tt % 2]
                q_dec, k_dec, k2, v_sb, dec, sg = hd["q_dec"], hd["k_dec"], hd["k2"], hd["v_sb"], hd["dec"], hd["sg"]
                O = linattn_core(tt, q_dec, k_dec, k2, v_sb, dec, Sf, Sb, qkT_sb, sT_sb)
                head_rms(O, o_sb, osq, ssq, rs)
                if d_dbg is not None and tt == build_program.dbg_tile:
                    dump(la, 256); dump(eb, 256); dump(q_dec, 256); dump(k_dec, 256)
                    dump(sT_sb.re("p h f -> p (h f)"), 512); dump(o_sb, 512); dump(v_sb, 512)
                k.tt("dve", o_sb.re("p (h f) -> p h f", h=4), o_sb.re("p (h f) -> p h f", h=4),
                     rs.bc(2, [128, 4, 128]), ALU.mult)
                k.tt("pool", o_tok, o_sb, sg, ALU.mult)
                if d_dbg is not None and tt == build_program.dbg_tile:
                    dump(o_tok, 512)
                out_proj(tt, o_tok, 4, wo, 0, gate1, oT_sb)

            if NTB:
                interleave(lambda: gla_A(0), [1, 2, 3], None, [4, 5, 6, 7])
            for tt in range(NTB):
                interleave((lambda t=tt: gla_A(t + 1)) if tt + 1 < NTB else None, [1, 2, 3],
                           lambda t=tt: gla_B(t), [4, 5, 6, 7])
            S.barrier()
            A.release(mB)
            qT = A.alloc([4, L], BF16, "qT")
            kT = A.alloc([4, L], BF16, "kT")
            vA = A.alloc([NT, 4, 129], BF16, "vA")
            qmax = A.alloc([8], F32, "qmax")
            kmax = A.alloc([8], F32, "kmax")
            k.memset("dve", qmax, 0.0)
            k.memset("dve", kmax, 0.0)
            k.memset("pool", vA[:, :, :, 128:129], 1.0)
            mA = A.mark()
            wd = A.alloc([KC, 1536], BF16, "wd")
            wdv = d_ewin.re("(kc p) n -> p kc n", p=128)
            for g in range(3):
                k.dma("pool", wd[:, :, g * 512:(g + 1) * 512], wdv[:, :, 1552 + g * 512:1552 + (g + 1) * 512])
            q_sb = A.alloc([512], F32, "q_sb")
            k_sb = A.alloc([512], F32, "k_sb")
            q_rot = A.alloc([512], BF16, "q_rot")
            k_rot = A.alloc([512], BF16, "k_rot")
            rt = [A.alloc([256], F32, "rt%d" % i) for i in range(4)]
            sqt = A.alloc([512], F32, "sqt")
            nrm = A.alloc([8], F32, "nrm")
            for tt in range(NT):
                h_t = hT_tile(tt)
                Z = [PS.next() for _ in range(3)]
                for g in range(3):
                    for kc in range(KC):
                        k.mm(Z[g], h_t[:, kc, :], wd[:, kc, g * 512:(g + 1) * 512],
                             start=(kc == 0), stop=(kc == KC - 1))
                k.copy("act", vA[:, tt, :, 0:128], Z[2].re("p (h f) -> p h f", h=4))
                for (zz, sb, rot, dstT, mx) in ((Z[0], q_sb, q_rot, qT, qmax), (Z[1], k_sb, k_rot, kT, kmax)):
                    k.copy("act", sb, zz)
                    k.tt("pool", sqt, sb, sb, ALU.mult)
                    k.red("dve", nrm, sqt.re("p (g f) -> p g f", g=8), ALU.add)
                    k.tt("dve", mx, mx, nrm, ALU.max)
                    rope(rot, sb, tt, rt)
                    TP = PS.next()
                    TPb = TP.bitcast(BF16)
                    for h in range(4):
                        k.tr(TPb[:, h * 128:(h + 1) * 128], rot[:, h * 128:(h + 1) * 128], identb)
                    k.copy("act", dstT[:, :, tt * 128:(tt + 1) * 128],
                           TPb[:, 0:512].re("p (h f) -> p h f", h=4))
            S.barrier()
            A.release(mA)
            wo2 = A.alloc([4, D], BF16, "wo2")
            k.dma("pool", wo2, d_ewout.re("(kc p) n -> p kc n", p=128)[:, 4:8, :])
            dgb = A.alloc([128], F32, "dgb")
            load_bc_row(dgb, d_diffg, 128)
            lam_init = 0.8 - 0.6 * math.exp(-0.3 * l)
            k.ts("dve", dgb, dgb, 1.0 - lam_init, ALU.mult)
            lin = A.alloc([256], F32, "lin")
            k.dma("sp", lin[0:1, :], d_lam)
            lp = A.alloc([128], F32, "lp")
            k.tt("dve", lp[0:1, :], lin[0:1, 0:128], lin[0:1, 128:256], ALU.mult)
            ls = A.alloc([2], F32, "ls")
            k.red("dve", ls[0:1, :], lp[0:1, :].re("p (g f) -> p g f", g=2), ALU.add)
            k.act(ls[0:1, :], ls[0:1, :], AF.Exp)
            nl = A.alloc([1], F32, "nl")
            k.tt("dve", nl[0:1, :], ls[0:1, 1:2], ls[0:1, 0:1], ALU.subtract)
            k.ts("dve", nl[0:1, :], nl[0:1, :], -lam_init, ALU.add)
            PL = PS.next()
            k.mm(PL[:, 0:1], ones[0:1, :], nl[0:1, 0:1])
            neglam = A.alloc([1], F32, "neglam")
            k.copy("dve", neglam, PL[:, 0:1])
            PQ = PS.next()
            k.tr(PQ[0:8, 0:128], qmax, ident)
            k.tr(PQ[0:8, 128:256], kmax, ident)
            m2 = A.alloc([2], F32, "m2")
            k.red("dve", m2[0:8, :], PQ[0:8, 0:256].re("p (g f) -> p g f", g=2), ALU.max)
            mm_ = A.alloc([1], F32, "mm_")
            k.tt("dve", mm_[0:8, :], m2[0:8, 0:1], m2[0:8, 1:2], ALU.mult)
            k.act(mm_[0:8, :], mm_[0:8, :], AF.Sqrt)
            dg = A.alloc([8], F32, "dg")
            k.ts("dve", dg[0:8, :], ident[0:8, 0:8], mm_[0:8, 0:1], ALU.mult, -0.125, ALU.mult)
            PM = PS.next()
            k.mm(PM[:, 0:8], ones[0:8, :], dg[0:8, :])
            negM = A.alloc([8], F32, "negM")
            k.copy("dve", negM, PM[:, 0:8])
            res = [dict(PT=[A.alloc([512], BF16, "PT%d_%d" % (s_, i)) for i in range(2)],
                        oa=A.alloc([128], F32, "oa%d" % s_), od=A.alloc([128], F32, "od%d" % s_),
                        r01=A.alloc([2], F32, "r01%d" % s_), dsq=A.alloc([128], F32, "dsq%d" % s_),
                        dss=A.alloc([1], F32, "dss%d" % s_), npt=0, pend=[],
                        acc=[PS.fixed(2 * s_), PS.fixed(2 * s_ + 1)]) for s_ in range(2)]
            o_tok2 = A.alloc([512], BF16, "o_tok2")
            o_tok2_h = [V(o_tok2.ap[:, h * 128:(h + 1) * 128], Buf("o_tok2_%d" % h)) for h in range(4)]
            oT2 = A.alloc([8, 128], BF16, "oT2")
            PS.set_rot([4, 5, 6, 7])

            def head_block(i, h, R):
                PT, oa, od, r01, dsq, dss, acc, pend = (R["PT"], R["oa"], R["od"], R["r01"], R["dsq"],
                                                        R["dss"], R["acc"], R["pend"])

                def flush_pv():
                    while pend:
                        pend.pop(0)()
                for m in range(2):
                    hm = h * 2 + m
                    for jb in range(0, i + 1, 4):
                        nj = min(4, i + 1 - jb)
                        SB = PS.next()
                        for jj in range(nj):
                            j = jb + jj
                            k.mm(SB[:, jj * 128:(jj + 1) * 128],
                                 kT[m * 64:(m + 1) * 64, h, j * 128:(j + 1) * 128],
                                 qT[m * 64:(m + 1) * 64, h, i * 128:(i + 1) * 128])
                        flush_pv()
                        P = PT[R["npt"] % 2]
                        R["npt"] += 1
                        k.act(P[:, 0:nj * 128], SB[:, 0:nj * 128], AF.Exp, scale=0.125,
                              bias=negM[:, hm:hm + 1])
                        if jb + nj == i + 1:
                            dsl = P[:, (nj - 1) * 128:nj * 128]
                            k.tt("pool", dsl, dsl, trib, ALU.mult)

                        def pv(P=P, nj=nj, jb=jb, m=m):
                            for jj in range(nj):
                                j = jb + jj
                                k.mm(acc[m][:, 0:129], P[:, jj * 128:(jj + 1) * 128], vA[:, j, h, :],
                                     start=(j == 0), stop=(j == i))
                        pend.append(pv)
                flush_pv()
                k.recip(r01[:, 0:1], acc[0][:, 128:129])
                k.recip(r01[:, 1:2], acc[1][:, 128:129])
                k.tt("dve", r01[:, 1:2], r01[:, 1:2], neglam, ALU.mult)
                k.ts("dve", oa, acc[0][:, 0:128], r01[:, 0:1], ALU.mult)
                k.stt(od, acc[1][:, 0:128], r01[:, 1:2], oa, ALU.mult, ALU.add)
                k.tt("pool", dsq, od, od, ALU.mult)
                k.red("dve", dss, dsq, ALU.add)
                k.ts("dve", dss, dss, 1.0 / 128.0, ALU.mult, EPS, ALU.add)
                k.tt("pool", dss, dss, mhalf[:, 0:1], ALU.pow)
                k.stt(o_tok2_h[h], od, dss[:, 0:1], dgb, ALU.mult, ALU.mult)

            o_tok2_all = V(o_tok2.ap, None)
            for i in range(0 if "nodiff" in build_program.stages else NT):
                interleave(lambda i=i: (head_block(i, 0, res[0]), head_block(i, 2, res[0])), [4, 5],
                           lambda i=i: (head_block(i, 1, res[1]), head_block(i, 3, res[1])), [6, 7])
                out_proj(i, o_tok2, 4, wo2, 0, gate1, oT2, src_bufs=o_tok2_h)
            S.barrier()
            PS.set_rot(range(8))
            A.release(mE)

        def odd_mixer(l):
            gate1 = mod_cols(l, 2)
            mO = A.mark()
            PS.set_rot(range(8))
            norm_all(mod_cols(l, 1), mod_cols(l, 0))
            wi = A.alloc([KC, 2560], BF16, "wi")
            wv = d_owin.re("(kc p) n -> p kc n", p=128)
            for g in range(5):
                k.dma("pool", wi[:, :, g * 512:(g + 1) * 512], wv[:, :, g * 512:(g + 1) * 512])
            wo = A.alloc([KC, D], BF16, "wo")
            k.dma("pool", wo, d_owout.re("(kc p) n -> p kc n", p=128))
            lngb = A.alloc([512], F32, "lngb")
            lnbb = A.alloc([512], F32, "lnbb")
            load_bc_row(lngb, d_lng, 512)
            load_bc_row(lnbb, d_lnb, 512)
            wsb = A.alloc([4, 128], BF16, "wsb")
            mW = A.mark()
            wsf = A.alloc([4, 128], F32, "wsf")
            k.dma("sp", wsf, d_swT.re("g j i -> j g i"))
            k.tt("dve", wsb, wsf, tri.bc(1, [128, 4, 128]), ALU.mult)
            S.barrier()
            A.release(mW)
            bsT = A.alloc([4], F32, "bsT")
            k.dma("sp", bsT, d_sbT)
            p1 = A.alloc([1], F32, "p1")
            k.ts("dve", p1, iof[:, 0:1], -1.0, ALU.mult, 1.0, ALU.add)
            EB = A.alloc([4, 64], F32, "EB")
            ENB = A.alloc([4, 64], F32, "ENB")
            E2 = A.alloc([4, 64], F32, "E2")
            dec = A.alloc([4], F32, "dec")
            tmpc = A.alloc([64], F32, "tmpc")
            for h in range(4):
                lg = math.log(1.0 - 2.0 ** (-5.0 - h))
                k.ts("dve", tmpc, ones[:, 0:64], p1[:, 0:1], ALU.mult)
                k.act(EB[:, h, :], tmpc, AF.Exp, scale=lg)
                k.ts("dve", EB[:, h, :], EB[:, h, :], 0.125, ALU.mult)
                k.act(ENB[:, h, :], tmpc, AF.Exp, scale=-lg)
                k.ts("dve", tmpc, tmpc, -1.0, ALU.mult, 128.0, ALU.add)
                k.act(E2[:, h, :], tmpc, AF.Exp, scale=lg)
                k.memset("dve", dec[:, h:h + 1], math.exp(lg * 128.0))
            EBv, ENBv, E2v = [t.re("p h f -> p (h f)") for t in (EB, ENB, E2)]
            Sf = A.alloc([4, 128], F32, "Sf")
            Sb = A.alloc([4, 128], BF16, "Sb")
            u = A.alloc([512], F32, "u")
            gv = A.alloc([512], F32, "gv")
            st6 = A.alloc([6], F32, "st6")
            mv = A.alloc([2], F32, "mv")
            rs1 = A.alloc([1], F32, "rs1")
            nmr = A.alloc([1], F32, "nmr")
            svn = A.alloc([512], BF16, "svn")
            qk_sb = A.alloc([512], F32, "qk_sb")
            qk_rot = A.alloc([512], F32, "qk_rot")
            rt2 = [A.alloc([256], F32, "rt%d" % i) for i in range(2)]
            rt = [rt2[0], rt2[1], rt2[0], rt2[1]]
            qkT_sb = A.alloc([8, 128], BF16, "qkT_sb")
            sT_sb = A.alloc([4, 128], BF16, "sT_sb")
            o_sb = A.alloc([512], F32, "o_sb")
            osq = A.alloc([512], F32, "osq")
            ssq = A.alloc([4], F32, "ssq")
            rs = A.alloc([4], F32, "rs")
            oT_sb = A.alloc([8, 128], BF16, "oT_sb")
            hand = [dict(q_dec=A.alloc([256], BF16, "q_dec%d" % i), k_dec=A.alloc([256], BF16, "k_dec%d" % i),
                         k2=A.alloc([256], BF16, "k2%d" % i), v_sb=A.alloc([512], BF16, "v_sb%d" % i),
                         sg=A.alloc([512], F32, "sg%d" % i), o_tok=A.alloc([1024], BF16, "o_tok%d" % i))
                    for i in range(2)]

            def odd_A(tt):
                hd = hand[tt % 2]
                q_dec, k_dec, k2, v_sb, sg, o_tok = (hd["q_dec"], hd["k_dec"], hd["k2"], hd["v_sb"],
                                                     hd["sg"], hd["o_tok"])
                h_t = hT_tile(tt)
                Z = []
                for g in range(4):
                    Z.append(PS.next())
                    for kc in range(KC):
                        k.mm(Z[g], h_t[:, kc, :], wi[:, kc, g * 512:(g + 1) * 512],
                             start=(kc == 0), stop=(kc == KC - 1))
                k.act(u, Z[0], AF.Gelu)
                k.act(gv, Z[1], AF.Gelu)
                k.copy("act", qk_sb, Z[2])
                k.copy("act", v_sb, Z[3])
                Z4 = PS.next()
                for kc in range(KC):
                    k.mm(Z4, h_t[:, kc, :], wi[:, kc, 2048:2560], start=(kc == 0), stop=(kc == KC - 1))
                k.act(sg, Z4, AF.Silu)
                S.op("dve", lambda e: e.bn_stats(out=st6.ap, in_=gv.ap), reads=[gv.buf], writes=[st6.buf])
                S.op("dve", lambda e: e.bn_aggr(out=mv.ap, in_=st6.ap), reads=[st6.buf], writes=[mv.buf])
                k.ts("dve", rs1, mv[:, 1:2], EPS, ALU.add)
                k.tt("pool", rs1, rs1, mhalf[:, 0:1], ALU.pow)
                k.stt(nmr, mv[:, 0:1], -1.0, rs1, ALU.mult, ALU.mult)
                k.act(gv, gv, AF.Identity, scale=rs1[:, 0:1], bias=nmr[:, 0:1])
                k.tt("pool", gv, gv, lngb, ALU.mult)
                k.tt("pool", svn, gv, lnbb, ALU.add)
                SG = PS.next()
                for g in range(4):
                    k.mm(SG[:, g * 128:(g + 1) * 128], wsb[:, g, :], svn[:, g * 128:(g + 1) * 128])
                for g in range(4):
                    k.stt(o_tok[:, g * 128:(g + 1) * 128], SG[:, g * 128:(g + 1) * 128], bsT[:, g:g + 1],
                          u[:, g * 128:(g + 1) * 128], ALU.add, ALU.mult)
                rope(qk_rot, qk_sb, tt, rt)
                k.tt("dve", q_dec, qk_rot[:, 0:256], EBv, ALU.mult)
                k.tt("pool", k_dec, qk_rot[:, 256:512], ENBv, ALU.mult)
                k.tt("dve", k2, qk_rot[:, 256:512], E2v, ALU.mult)

            def odd_B(tt):
                hd = hand[tt % 2]
                q_dec, k_dec, k2, v_sb, sg, o_tok = (hd["q_dec"], hd["k_dec"], hd["k2"], hd["v_sb"],
                                                     hd["sg"], hd["o_tok"])
                O = linattn_core(tt, q_dec, k_dec, k2, v_sb, dec, Sf, Sb, qkT_sb, sT_sb)
                head_rms(O, o_sb, osq, ssq, rs)
                k.tt("dve", o_sb.re("p (h f) -> p h f", h=4), o_sb.re("p (h f) -> p h f", h=4),
                     rs.bc(2, [128, 4, 128]), ALU.mult)
                k.tt("pool", o_tok[:, 512:1024], o_sb, sg, ALU.mult)
                out_proj(tt, o_tok, 8, wo, 0, gate1, oT_sb)

            interleave(lambda: odd_A(0), [0, 1, 2, 3], None, [4, 5, 6, 7])
            for tt in range(NT):
                interleave((lambda t=tt: odd_A(t + 1)) if tt + 1 < NT else None, [0, 1, 2, 3],
                           lambda t=tt: odd_B(t), [4, 5, 6, 7])
            S.barrier()
            A.release(mO)

        def moe(l):
            gate2 = mod_cols(l, 5)
            mM = A.mark()
            PS.set_rot(range(8))
            GT = A.alloc([L], F32, "GT")
            bo = A.alloc([D], F32, "bo")
            k.dma("sp", bo[0:NE, :], d_ebo[l])
            biT = A.alloc([NE, 16], F32, "biT")
            k.dma("sp", biT, d_ebi[l])
            bi1 = A.alloc([NE, 8], F32, "bi1")
            k.ts("dve", bi1, biT[:, :, 8:16], 1.0, ALU.add)
            ring = [[A.alloc([KC, 512], BF16, "wr%d_%d" % (s, i)) for i in range(2)] +
                    [A.alloc([4, D], BF16, "wr%d_2" % s)] for s in range(2)]
            wiv = [d_ewi[l, e].re("(kc p) n -> p kc n", p=128) for e in range(NE)]
            wov = [d_ewo[l, e].re("(kc p) n -> p kc n", p=128) for e in range(NE)]

            def load_half(he):
                e, hh = divmod(he, 2)
                s = he % 2
                k.dma("pool", ring[s][0], wiv[e][:, :, hh * 512:(hh + 1) * 512])
                k.dma("pool", ring[s][1], wiv[e][:, :, D + hh * 512:D + (hh + 1) * 512])
                k.dma("pool", ring[s][2], wov[e][:, hh * 4:(hh + 1) * 4, :])

            load_half(0)
            load_half(1)
            m1 = A.mark()
            rw = A.alloc([KC, NE], F32, "rw")
            k.dma("sp", rw, d_rw[l].re("(kc p) e -> p kc e", p=128))
            rb = A.alloc([NE], F32, "rb")
            k.dma("sp", rb[0:1, :], d_rb[l])
            hf = A.alloc([KC, 512], F32, "hf")
            lg4 = [A.alloc([4, NE], F32, "lg4_%d" % i) for i in range(2)]
            t84 = [A.alloc([4, 8], F32, "t84_%d" % i) for i in range(2)]
            ex4 = [A.alloc([4, NE], F32, "ex4_%d" % i) for i in range(2)]
            msk4 = [A.alloc([4, NE], F32, "msk4_%d" % i) for i in range(2)]
            sm4 = [A.alloc([4], F32, "sm4_%d" % i) for i in range(2)]
            ntm = norm_alloc()

            def router_mm(q):
                LG = PS.next()
                for r in range(4):
                    for kc in range(KC):
                        k.mm(LG[:, r * NE:(r + 1) * NE], hf[:, kc, r * 128:(r + 1) * 128], rw[:, kc, :],
                             start=(kc == 0), stop=False)
                    k.mm(LG[:, r * NE:(r + 1) * NE], ones[0:1, :], rb[0:1, :], start=False, stop=True)
                k.copy("act", lg4[q % 2], LG[:, 0:4 * NE].re("p (r e) -> p r e", r=4))

            def route_elem(q):
                lg, t8, ex, msk, sm = lg4[q % 2], t84[q % 2], ex4[q % 2], msk4[q % 2], sm4[q % 2]
                for r in range(4):
                    S.op("dve", lambda e, r=r: e.max(out=t8.ap[:, r, :], in_=lg.ap[:, r, :]),
                         reads=[lg.buf], writes=[t8.buf])
                k.tt("dve", ex, lg, V(t8.ap[:, :, 0:1].broadcast_to([128, 4, NE]), t8.buf), ALU.subtract)
                k.act(ex, ex, AF.Exp)
                k.tt("dve", msk, lg, V(t8.ap[:, :, 3:4].broadcast_to([128, 4, NE]), t8.buf), ALU.is_ge)
                k.tt("dve", ex, ex, msk, ALU.mult)
                k.red("dve", sm, ex, ALU.add)
                k.recip(sm, sm)
                k.tt("dve", ex, ex, sm.bc(2, [128, 4, NE]), ALU.mult)
                TG = PS.next()
                for r in range(4):
                    k.tr(TG[0:NE, r * 128:(r + 1) * 128], ex[:, r, :], ident)
                k.copy("act", GT[0:NE, q * 512:(q + 1) * 512], TG[0:NE, :])

            norm_quad(0, mod_cols(l, 4), mod_cols(l, 3), hT[0], out_f32=hf, nt=ntm)
            for q in range(NQ):
                router_mm(q)
                if q + 1 < NQ:
                    norm_quad(q + 1, mod_cols(l, 4), mod_cols(l, 3), hT[q + 1], out_f32=hf, nt=ntm)
                route_elem(q)
            GTb = A.alloc([L], BF16, "GTb")
            k.copy("dve", GTb[0:NE, :], GT[0:NE, :])
            k.dma("sp", d_gtb[l], GTb[0:NE, :])
            S.barrier()
            A.release(m1)
            for q in range(NQ):
                for dc in range(KC):
                    Y = PS.next()
                    k.mm(Y, bo[0:NE, dc * 128:(dc + 1) * 128], GT[0:NE, q * 512:(q + 1) * 512])
                    k.stt(xT[dc][q], Y, gate2[dc], xT[dc][q], ALU.mult, ALU.add)
            aT = [A.alloc([4, 512], BF16, "aT%d" % i) for i in range(2)]
            gate_bc = [A.alloc([L], BF16, "gate_bc%d" % i) for i in range(2)]

            def load_gate(e):
                src = d_gtb[l][e:e + 1, :]
                k.dma("sp", gate_bc[e % 2],
                      V(src.ap.partition_broadcast(128).rearrange("p o n -> p (o n)"), src.buf))
            gt = [A.alloc([512], F32, "g%d" % i) for i in range(2)]
            sgt = [A.alloc([512], F32, "s%d" % i) for i in range(2)]
            lt = [A.alloc([512], F32, "l%d" % i) for i in range(2)]
            ut = [A.alloc([512], F32, "u%d" % i) for i in range(2)]
            PS.set_rot([0, 1, 2, 3])
            ybank = [PS.fixed(4), PS.fixed(5), PS.fixed(6), PS.fixed(7)]
            load_gate(0)
            load_gate(1)
            state = {"cnt": 0, "ny": 0}

            def z_part(n, he, q):
                e, hh = divmod(he, 2)
                w_glu, w_lin, w_o = ring[he % 2]
                gs_ = gate_bc[e % 2][:, q * 512:(q + 1) * 512]
                a_ = aT[n % 2]
                pend = None
                for fc in range(4):
                    i3 = state["cnt"] % 2
                    state["cnt"] += 1
                    ZG_ = PS.next()
                    for kc in range(KC):
                        k.mm(ZG_, w_glu[:, kc, fc * 128:(fc + 1) * 128], hT[q][:, kc, :],
                             start=(kc == 0), stop=(kc == KC - 1))
                    ZL_ = PS.next()
                    for kc in range(KC):
                        k.mm(ZL_, w_lin[:, kc, fc * 128:(fc + 1) * 128], hT[q][:, kc, :],
                             start=(kc == 0), stop=(kc == KC - 1))
                    col = hh * 4 + fc
                    k.ts("dve", gt[i3], ZG_, biT[:, e, col:col + 1], ALU.add, 7.0, ALU.min)
                    k.act(sgt[i3], gt[i3], AF.Sigmoid, scale=1.702)
                    k.tt("pool", gt[i3], gt[i3], sgt[i3], ALU.mult)
                    k.act(lt[i3], ZL_, AF.Identity, bias=bi1[:, e, col:col + 1])
                    k.ts("dve", lt[i3], lt[i3], -6.0, ALU.max, 8.0, ALU.min)
                    if pend is not None:
                        pf, pi = pend
                        k.tt("dve", ut[pi], gt[pi], lt[pi], ALU.mult)
                        k.tt("pool", a_[:, pf, :], ut[pi], gs_, ALU.mult)
                    pend = (fc, i3)
                pf, pi = pend
                k.tt("dve", ut[pi], gt[pi], lt[pi], ALU.mult)
                k.tt("pool", a_[:, pf, :], ut[pi], gs_, ALU.mult)

            def y_part(n, he, q):
                w_glu, w_lin, w_o = ring[he % 2]
                a_ = aT[n % 2]
                for dc in range(KC):
                    Y = ybank[state["ny"] % 4]
                    state["ny"] += 1
                    for fc in range(4):
                        k.mm(Y, w_o[:, fc, dc * 128:(dc + 1) * 128], a_[:, fc, :],
                             start=(fc == 0), stop=(fc == 3))
                    k.stt(xT[dc][q], Y, gate2[dc], xT[dc][q], ALU.mult, ALU.add)

            steps = [(he, q) for he in range(2 * NE) for q in range(NQ)]
            for n, (he, q) in enumerate(steps):
                z_part(n, he, q)
                if n > 0:
                    y_part(n - 1, *steps[n - 1])
                if q == 1 and he >= 1 and he + 1 < 2 * NE:
                    load_half(he + 1)
                if q == 1 and he % 2 == 0 and he >= 2 and he // 2 + 1 < NE:
                    load_gate(he // 2 + 1)
            y_part(len(steps) - 1, *steps[-1])
            S.barrier()
            PS.set_rot(range(8))
            A.release(mM)

        def final_out():
            mF = A.mark()
            PS.set_rot(range(8))
            hf = A.alloc([KC, 512], F32, "hfin")
            dummy = A.alloc([KC, 512], BF16, "dummyb")
            zero = A.alloc([1], F32, "zero")
            k.memset("dve", zero, 0.0)
            ot = [A.alloc([D], F32, "ot%d" % i) for i in range(2)]
            gcols = [fgT[:, c:c + 1] for c in range(KC)]
            zcols = [zero[:, 0:1] for c in range(KC)]
            ntf = norm_alloc()
            for q in range(NQ):
                norm_quad(q, gcols, zcols, dummy, out_f32=hf, nt=ntf)
                for r in range(4):
                    tt = q * 4 + r
                    o = ot[tt % 2]
                    for half in range(2):
                        pb = PS.next()
                        for cc in range(4):
                            c = half * 4 + cc
                            k.tr(pb[:, cc * 128:(cc + 1) * 128], hf[:, c, r * 128:(r + 1) * 128], ident)
                        k.copy("act" if half else "dve", o[:, half * 512:(half + 1) * 512], pb)
                    k.dma("sp", d_out[tt * 128:(tt + 1) * 128, :], o)
            A.release(mF)

        def write_x_raw():
            mF = A.mark()
            PS.set_rot(range(8))
            ot = [A.alloc([D], F32, "ot%d" % i) for i in range(2)]
            for tt in range(NT if "nox" not in build_program.stages else 1):
                q, r = divmod(tt, 4)
                o = ot[tt % 2]
                for half in range(2):
                    pb = PS.next()
                    for cc in range(4):
                        c = half * 4 + cc
                        k.tr(pb[:, cc * 128:(cc + 1) * 128], xT[c][q][:, r * 128:(r + 1) * 128], ident)
                    k.copy("act" if half else "dve", o[:, half * 512:(half + 1) * 512], pb)
                k.dma("sp", d_out[tt * 128:(tt + 1) * 128, :], o)
            A.release(mF)

        prologue()
        for l in layers:
            if "mix" in stages:
                if l % 2 == 0:
                    even_mixer(l)
                else:
                    odd_mixer(l)
            if "moe" in stages:
                moe(l)
        if do_final:
            final_out()
        else:
            write_x_raw()
        S.barrier()
        nsem = S.emit()
        build_program.info = dict(nsem=nsem, nwaits=S.nwaits, nops=dict(S.nops), ndma=dict(S.ndma),
                                  peak_words=A.peak)
    return nc


build_program.stages = ("mix", "moe")
build_program.info = {}
build_program.dbg_tile = 0


def make_in_maps(inp, cores=range(N_CORES)):
    f = lambda a: np.ascontiguousarray(np.asarray(a))
    half = 32
    invf = (np.float32(10000.0) ** (-(np.arange(half, dtype=np.float32) / np.float32(half)))).astype(np.float32)
    shared = {
        "invf": f(np.broadcast_to(invf[None, :], (128, half))),
        "w_ada": f(inp["w_ada"]),
        "b_adaT": f(np.asarray(inp["b_ada"]).reshape(2, 48, 128).transpose(2, 0, 1)),
        "even_w_in": f(inp["even_w_in"][0]),
        "gla_w_gate": f(inp["gla_w_gate"][0]),
        "gla_b_gate": f(inp["gla_b_gate"]),
        "gla_norm_g": f(inp["gla_norm_g"]),
        "lam_in": f(np.concatenate([inp["diff_lam_q1"][0], inp["diff_lam_q2"][0],
                                    inp["diff_lam_k1"][0], inp["diff_lam_k2"][0]])[None, :]),
        "diff_norm_g": f(inp["diff_norm_g"]),
        "even_w_out": f(inp["even_w_out"][0]),
        "odd_w_in": f(inp["odd_w_in"][0]),
        "sgu_ln_g": f(inp["sgu_ln_g"]),
        "sgu_ln_b": f(inp["sgu_ln_b"]),
        "sgu_wT": f(np.asarray(inp["sgu_w"][0]).transpose(0, 2, 1)),
        "sgu_bT": f(np.asarray(inp["sgu_b"][0]).T),
        "odd_w_out": f(inp["odd_w_out"][0]),
        "router_w": f(inp["router_w"]),
        "router_b": f(np.asarray(inp["router_b"])[:, None, :]),
        "expert_w_in": f(inp["expert_w_in"]),
        "expert_b_inT": f(np.asarray(inp["expert_b_in"]).reshape(2, NE, 16, 128).transpose(0, 3, 1, 2)),
        "expert_w_out": f(inp["expert_w_out"]),
        "expert_b_out": f(inp["expert_b_out"]),
        "final_gT": f(np.asarray(inp["final_norm_g"]).reshape(KC, 128).T),
    }
    maps = []
    for b in cores:
        m = dict(shared)
        m["x"] = f(inp["x"][b])
        m["cT"] = f(np.asarray(inp["c"][b]).reshape(KC, 128).T)
        m["posT"] = f(np.asarray(inp["positions"][b]).astype(np.int32).reshape(NT, 128).T)
        maps.append({n: m[n] for n in build_program.in_names})
    return maps


def kernel(**inputs):
    build_program.stages = ("mix", "moe")
    nc = build_program(layers=(0, 1), do_final=True)
    maps = make_in_maps(inputs)
    res = run_bass_kernel_spmd(nc, maps, core_ids=list(range(N_CORES)))
    return np.stack([np.asarray(r["yout"]) for r in res.results], axis=0).astype(np.float32)
```

```python
from contextlib import ExitStack
import math
import threading
import numpy as np
import concourse.bass as bass
import concourse.mybir as mybir
from concourse.bass_utils import run_bass_kernel_spmd

F32 = mybir.dt.float32
BF16 = mybir.dt.bfloat16
I32 = mybir.dt.int32
AF = mybir.ActivationFunctionType
ALU = mybir.AluOpType
AX = mybir.AxisListType

D = 1024
L = 2048
NT = 16
NQ = 4
KC = 8
NE = 32
EPS = 1e-6
N_CORES = 8


class Buf:
    __slots__ = ("name", "w", "r", "excl")

    def __init__(self, name="", excl=False):
        self.name = name
        self.w = None
        self.r = {}
        self.excl = excl


class Sched:
    ENGS = ("pe", "act", "dve", "pool", "sp")
    LIMIT = 30000
    NSLOT = 8

    def __init__(self, nc, stack):
        self.nc = nc
        self.stack = stack
        self.eng = {"pe": nc.tensor, "act": nc.scalar, "dve": nc.vector,
                    "pool": nc.gpsimd, "sp": nc.sync}
        self.ops = []
        self.nops = {e: 0 for e in self.ENGS}
        self.seen = {e: {} for e in self.ENGS}
        self.ndma = {e: 0 for e in self.ENGS}
        self.marked = set()
        self.last_dma = {}
        self.il = None

    def _deps(self, eng, reads, writes, skip_same):
        deps = {}

        def add(tok):
            if tok is None:
                return
            k, i = tok
            if skip_same and k == eng:
                return
            if deps.get(k, -1) < i:
                deps[k] = i
        for b in reads:
            add(b.w)
        for b in writes:
            add(b.w)
            for k, i in b.r.items():
                add((k, i))
        seen = self.seen[eng]
        waits = []
        for k, i in deps.items():
            if seen.get(k, -1) >= i:
                continue
            seen[k] = i
            waits.append((k, i))
            self.marked.add((k, i))
        return waits

    def _update(self, tok, reads, writes):
        k, i = tok
        for b in reads:
            if b.r.get(k, -1) < i:
                b.r[k] = i
        for b in writes:
            b.w = tok
            b.r = {}

    def op(self, eng, fn, reads=(), writes=()):
        il = self.il
        if il is not None:
            il.acquire()
        try:
            return self._op(eng, fn, reads, writes)
        finally:
            if il is not None:
                il.release()

    def _op(self, eng, fn, reads=(), writes=()):
        ex = [b for b in reads if b.excl]
        if ex:
            writes = list(writes) + [b for b in ex if b not in writes]
            reads = [b for b in reads if not b.excl]
        waits = self._deps(eng, reads, writes, eng == "pe")
        idx = self.nops[eng]
        self.nops[eng] += 1
        tok = (eng, idx)
        self.ops.append((eng, fn, waits, "c", eng, idx))
        self._update(tok, reads, writes)
        return tok

    def dma(self, eng, fn, reads=(), writes=()):
        assert self.il is None, "no DMA inside interleaved sections"
        n = self.ndma[eng]
        self.ndma[eng] += 1
        slot = n % self.NSLOT
        key = ("d", eng, slot)
        idx = n // self.NSLOT
        waits = self._deps(eng, reads, writes, False)
        if idx > 0:
            seen = self.seen[eng]
            if seen.get(key, -1) < idx - 1:
                seen[key] = idx - 1
                waits.append((key, idx - 1))
        tok = (key, idx)
        self.last_dma[key] = idx
        self.ops.append((eng, fn, waits, "d", key, idx))
        self._update(tok, reads, writes)
        return tok

    def barrier(self):
        if "nobar" in build_program.stages:
            return
        toks = []
        for e in self.ENGS:
            if self.nops[e] > 0:
                toks.append((e, self.nops[e] - 1))
        for key, idx in self.last_dma.items():
            toks.append((key, idx))
        for e in self.ENGS:
            seen = self.seen[e]
            waits = []
            for (k, i) in toks:
                if seen.get(k, -1) >= i:
                    continue
                seen[k] = i
                waits.append((k, i))
                self.marked.add((k, i))
            if waits:
                self.ops.append((e, None, waits, "w", None, None))

    def wait_all(self, eng, bufs):
        waits = self._deps(eng, bufs, (), False)
        self.ops.append((eng, None, waits, "w", None, None))

    def emit(self):
        nc = self.nc
        sems = {}

        def get_sem(name):
            if name not in sems:
                sems[name] = self.stack.enter_context(nc.semaphore(name))
            return sems[name]

        val = {}
        counters = {e: 0 for e in self.ENGS}
        for (eng, fn, waits, kind, key, idx) in self.ops:
            if kind == "c" and (key, idx) in self.marked:
                c = counters[eng]
                counters[eng] += 1
                val[(key, idx)] = ("s_%s_%d" % (eng, c // self.LIMIT), (c % self.LIMIT) + 1)
        nw = 0
        for (eng, fn, waits, kind, key, idx) in self.ops:
            e = self.eng[eng]
            for (k, i) in waits:
                if isinstance(k, tuple):
                    sname = "d_%s_%d" % (k[1], k[2])
                    v = 16 * (i + 1)
                else:
                    sname, v = val[(k, i)]
                e.wait_ge(get_sem(sname), v)
                nw += 1
            if fn is None:
                continue
            ins = fn(e)
            if kind == "d":
                ins.then_inc(get_sem("d_%s_%d" % (key[1], key[2])), 16)
            elif (key, idx) in self.marked:
                ins.then_inc(get_sem(val[(key, idx)][0]), 1)
        self.nwaits = nw
        return len(sems)


class Interleaver:
    def __init__(self):
        self.cv = threading.Condition()
        self.turn = 0
        self.alive = [True, True]
        self.ids = {}

    def register(self, i):
        self.ids[threading.get_ident()] = i

    def acquire(self):
        me = self.ids[threading.get_ident()]
        with self.cv:
            while self.turn != me:
                self.cv.wait()

    def release(self):
        me = self.ids[threading.get_ident()]
        with self.cv:
            if self.alive[1 - me]:
                self.turn = 1 - me
            self.cv.notify_all()

    def finish(self, i):
        with self.cv:
            self.alive[i] = False
            self.turn = 1 - i
            self.cv.notify_all()


class V:
    __slots__ = ("ap", "buf")

    def __init__(self, ap, buf=None, name=""):
        self.ap = ap
        self.buf = buf if buf is not None else Buf(name)

    def __getitem__(self, k):
        return V(self.ap[k], self.buf)

    def bitcast(self, dt):
        return V(self.ap.bitcast(dt), self.buf)

    def re(self, s, **kw):
        return V(self.ap.rearrange(s, **kw), self.buf)

    def bc(self, axis, shape):
        return V(self.ap.unsqueeze(axis).broadcast_to(shape), self.buf)


def _a(x):
    return x.ap if isinstance(x, V) else x


def _bufs(*xs):
    out = []
    for x in xs:
        if isinstance(x, V) and x.buf not in out:
            out.append(x.buf)
    return out


class K:
    def __init__(self, S):
        self.S = S

    def tt(self, eng, out, a, b, op):
        return self.S.op(eng, lambda e: e.tensor_tensor(out=out.ap, in0=a.ap, in1=b.ap, op=op),
                         reads=_bufs(a, b), writes=_bufs(out))

    def ts(self, eng, out, a, s1, op0, s2=None, op1=None):
        kw = {}
        if op1 is not None:
            kw["op1"] = op1
        return self.S.op(eng, lambda e: e.tensor_scalar(out=out.ap, in0=a.ap, scalar1=_a(s1), scalar2=_a(s2),
                                                        op0=op0, **kw),
                         reads=_bufs(a, s1, s2), writes=_bufs(out))

    def stt(self, out, a, s, b, op0, op1):
        return self.S.op("dve", lambda e: e.scalar_tensor_tensor(out=out.ap, in0=a.ap, scalar=_a(s), in1=b.ap,
                                                                 op0=op0, op1=op1),
                         reads=_bufs(a, s, b), writes=_bufs(out))

    def act(self, out, a, func, scale=1.0, bias=None, accum=None):
        kw = {}
        if bias is not None:
            kw["bias"] = _a(bias)
        if accum is not None:
            kw["accum_out"] = accum.ap
        return self.S.op("act", lambda e: e.activation(out=out.ap, in_=a.ap, func=func, scale=_a(scale), **kw),
                         reads=_bufs(a, scale, bias), writes=_bufs(out, accum))

    def copy(self, eng, out, a):
        if eng == "act":
            return self.S.op(eng, lambda e: e.copy(out=out.ap, in_=a.ap), reads=_bufs(a), writes=_bufs(out))
        return self.S.op(eng, lambda e: e.tensor_copy(out=out.ap, in_=a.ap), reads=_bufs(a), writes=_bufs(out))

    def memset(self, eng, out, val):
        return self.S.op(eng, lambda e: e.memset(out.ap, val), writes=_bufs(out))

    def mm(self, out, lhsT, rhs, start=True, stop=True):
        return self.S.op("pe", lambda e: e.matmul(out.ap, lhsT=lhsT.ap, rhs=rhs.ap, start=start, stop=stop),
                         reads=_bufs(lhsT, rhs), writes=_bufs(out))

    def tr(self, out, a, ident):
        return self.S.op("pe", lambda e: e.transpose(out.ap, a.ap, ident.ap),
                         reads=_bufs(a, ident), writes=_bufs(out))

    def red(self, eng, out, a, op, axis=AX.X):
        return self.S.op(eng, lambda e: e.tensor_reduce(out=out.ap, in_=a.ap, axis=axis, op=op),
                         reads=_bufs(a), writes=_bufs(out))

    def recip(self, out, a):
        return self.S.op("dve", lambda e: e.reciprocal(out=out.ap, in_=a.ap), reads=_bufs(a), writes=_bufs(out))

    def dma(self, eng, out, a, **kw):
        return self.S.dma(eng, lambda e: e.dma_start(out=out.ap, in_=a.ap, **kw),
                          reads=_bufs(a), writes=_bufs(out))


class Arena:
    def __init__(self, ap_f32, nwords):
        self.base = ap_f32
        self.n = nwords
        self.off = 0
        self.peak = 0

    def mark(self):
        return self.off

    def release(self, m):
        self.off = m

    def alloc(self, shape, dtype=F32, name="", parts=128):
        n = int(np.prod(shape))
        esz = 4 if dtype in (F32, I32) else 2
        words = (n * esz + 3) // 4
        assert self.off + words <= self.n, ("SBUF arena overflow", name, self.off, words, self.n)
        ap = self.base[0:parts, self.off:self.off + words]
        self.off += words
        self.peak = max(self.peak, self.off)
        if dtype != F32:
            ap = ap.bitcast(dtype)
        ap = ap[:, 0:n]
        if len(shape) > 1:
            names = " ".join("d%d" % i for i in range(len(shape)))
            kw = {"d%d" % i: int(s) for i, s in enumerate(shape)}
            ap = ap.rearrange("p (%s) -> p %s" % (names, names), **kw)
        return V(ap, Buf(name))


class PsPool:
    def __init__(self, banks):
        self.banks = banks
        self.rot = list(range(len(banks)))
        self.i = 0
        self.tl = {}

    def set_rot(self, idxs):
        self.rot = list(idxs)
        self.i = 0

    def next(self):
        st = self.tl.get(threading.get_ident())
        if st is not None:
            b = self.banks[st[0][st[1] % len(st[0])]]
            st[1] += 1
            return b
        b = self.banks[self.rot[self.i % len(self.rot)]]
        self.i += 1
        return b

    def fixed(self, i):
        return self.banks[i]


ARENA_WORDS = 52800


def build_program(layers=(0, 1), do_final=True, dbg=None):
    nc = bass.Bass("TRN2", target_bir_lowering=False)

    stages = build_program.stages
    in_names = []

    def din(name, shape, dt=F32, used=True):
        if not used:
            return None
        in_names.append(name)
        return V(nc.dram_tensor(name, list(shape), dt, kind="ExternalInput").ap(), name=name)

    has_mix0 = (0 in layers) and ("mix" in stages)
    has_mix1 = (1 in layers) and ("mix" in stages)
    has_moe = len(layers) > 0 and ("moe" in stages)
    has_mod = len(layers) > 0 and ("nomod" not in stages)
    d_x = din("x", [L, D])
    d_cT = din("cT", [128, KC])
    d_pos = din("posT", [128, NT], I32)
    d_invf = din("invf", [128, 32])
    d_wada = din("w_ada", [2, D, 6 * D], used=has_mod)
    d_bada = din("b_adaT", [128, 2, 48])
    d_ewin = din("even_w_in", [D, 3088], used=has_mix0)
    d_wg = din("gla_w_gate", [16, 256], used=has_mix0)
    d_bg = din("gla_b_gate", [1, 256], used=has_mix0)
    d_glag = din("gla_norm_g", [1, 128], used=has_mix0)
    d_lam = din("lam_in", [1, 256], used=has_mix0)
    d_diffg = din("diff_norm_g", [1, 128], used=has_mix0)
    d_ewout = din("even_w_out", [D, D], used=has_mix0)
    d_owin = din("odd_w_in", [D, 2560], used=has_mix1)
    d_lng = din("sgu_ln_g", [1, 512], used=has_mix1)
    d_lnb = din("sgu_ln_b", [1, 512], used=has_mix1)
    d_swT = din("sgu_wT", [4, 128, 128], used=has_mix1)
    d_sbT = din("sgu_bT", [128, 4], used=has_mix1)
    d_owout = din("odd_w_out", [D, D], used=has_mix1)
    d_rw = din("router_w", [2, D, NE], used=has_moe)
    d_rb = din("router_b", [2, 1, NE], used=has_moe)
    d_ewi = din("expert_w_in", [2, NE, D, 2 * D], used=has_moe)
    d_ebi = din("expert_b_inT", [2, 128, NE, 16], used=has_moe)
    d_ewo = din("expert_w_out", [2, NE, D, D], used=has_moe)
    d_ebo = din("expert_b_out", [2, NE, D], used=has_moe)
    d_fg = din("final_gT", [128, KC])
    build_program.in_names = in_names
    d_out = V(nc.dram_tensor("yout", [L, D], F32, kind="ExternalOutput").ap(), name="out")
    d_gtb = [V(nc.dram_tensor("gtb%d" % l, [NE, L], BF16, kind="Internal").ap(), name="gtb%d" % l) for l in range(2)]
    d_dbg = None
    if dbg is not None:
        d_dbg = V(nc.dram_tensor("dbg", [128, dbg], F32, kind="ExternalOutput").ap(), name="dbg")

    with ExitStack() as st:
        S = Sched(nc, st)
        k = K(S)
        big = st.enter_context(nc.sbuf_tensor("arena", [128, ARENA_WORDS], F32))
        A = Arena(big, ARENA_WORDS)
        banks = [V(st.enter_context(nc.psum_tensor("ps%d" % i, [128, 512], F32))[:, :], Buf("ps%d" % i, excl=True))
                 for i in range(8)]
        PS = PsPool(banks)

        def interleave(fA, rotA, fB, rotB):
            il = Interleaver()
            errs = []

            def wrap(i, f, rot):
                il.register(i)
                PS.tl[threading.get_ident()] = [list(rot), 0]
                try:
                    if f is not None:
                        f()
                except BaseException as ex:
                    errs.append(ex)
                finally:
                    PS.tl.pop(threading.get_ident(), None)
                    il.finish(i)
            S.il = il
            ts = [threading.Thread(target=wrap, args=(0, fA, rotA)),
                  threading.Thread(target=wrap, args=(1, fB, rotB))]
            for t in ts:
                t.start()
            for t in ts:
                t.join()
            S.il = None
            if errs:
                raise errs[0]

        xT_all = A.alloc([KC, L], F32, "xT")
        xT = [[V(xT_all.ap[:, c, q * 512:(q + 1) * 512], Buf("xT%d_%d" % (c, q))) for q in range(NQ)]
              for c in range(KC)]
        hT_all = A.alloc([KC, L], BF16, "hT")
        hT = [V(hT_all.ap[:, :, q * 512:(q + 1) * 512], Buf("hT%d" % q)) for q in range(NQ)]

        def hT_tile(tt):
            q, r = divmod(tt, 4)
            return hT[q][:, :, r * 128:(r + 1) * 128]

        ident = A.alloc([128], F32, "ident")
        identb = A.alloc([128], BF16, "identb")
        tri = A.alloc([128], F32, "tri")
        trib = A.alloc([128], BF16, "trib")
        tri16 = A.alloc([128], F32, "tri16")
        ones = A.alloc([128], F32, "ones")
        ones16 = A.alloc([128], F32, "ones16")
        mhalf = A.alloc([8], F32, "mhalf")
        mod = A.alloc([2, 48], F32, "mod")
        fgT = A.alloc([KC], F32, "fgT")
        cosT = A.alloc([NT, 32], F32, "cos")
        sinT = A.alloc([NT, 32], F32, "sin")

        io = A.alloc([128], I32, "io")
        S.op("pool", lambda e: e.iota(io.ap, pattern=[[1, 128]], base=0, channel_multiplier=-1),
             writes=[io.buf])
        iof = A.alloc([128], F32, "iof")
        k.copy("dve", iof, io)
        k.ts("dve", tri, iof, 0.0, ALU.is_ge)
        k.ts("dve", ident, iof, 0.0, ALU.is_equal)
        k.copy("dve", identb, ident)
        k.copy("dve", trib, tri)
        k.ts("dve", tri16, tri, 1.0 / 16.0, ALU.mult)
        k.memset("dve", ones, 1.0)
        k.memset("dve", ones16, 1.0 / 16.0)
        k.memset("dve", mhalf, -0.5)
        k.dma("sp", fgT, d_fg)

        dbg_off = [0]

        def dump(v, ncols, parts=128):
            if d_dbg is None:
                return
            m = A.mark()
            t = A.alloc([ncols], F32, "dbgt")
            k.copy("dve", t[0:parts, :], v)
            k.dma("sp", d_dbg[0:parts, dbg_off[0]:dbg_off[0] + ncols], t[0:parts, :])
            S.barrier()
            A.release(m)
            dbg_off[0] += ncols

        def prologue():
            m0 = A.mark()
            PS.set_rot(range(8))
            cT = A.alloc([KC], F32, "cT")
            k.dma("sp", cT, d_cT)
            cact = A.alloc([KC], F32, "cact")
            k.act(cact, cT, AF.Silu)
            badaT = A.alloc([2, 48], F32, "badaT")
            k.dma("sp", badaT, d_bada)
            wa = [A.alloc([KC, 512], F32, "wa%d" % i) for i in range(2)]
            row = A.alloc([6 * D], F32, "modrow")
            nld = 0
            for l in range(2):
                if l not in layers or "nomod" in build_program.stages:
                    continue
                wv = d_wada[l].re("(kc p) n -> p kc n", p=128)
                for g in range(12):
                    w = wa[nld % 2]
                    nld += 1
                    k.dma("sp", w, wv[:, :, g * 512:(g + 1) * 512])
                    pr = PS.next()
                    for kc in range(KC):
                        k.mm(pr[0:1, :], cact[:, kc:kc + 1], w[:, kc, :], start=(kc == 0), stop=(kc == KC - 1))
                    k.copy("act" if g % 2 else "dve", row[0:1, g * 512:(g + 1) * 512], pr[0:1, :])
                pm = PS.next()
                for j in range(48):
                    k.mm(pm[:, j:j + 1], row[0:1, j * 128:(j + 1) * 128], ones[0:1, 0:1])
                k.tt("dve", mod[:, l, :], pm[:, 0:48], badaT[:, l, :], ALU.add)
                k.ts("dve", mod[:, l, 8:16], mod[:, l, 8:16], 1.0, ALU.add)
                k.ts("dve", mod[:, l, 32:40], mod[:, l, 32:40], 1.0, ALU.add)
            xin = [A.alloc([D], F32, "xin%d" % i) for i in range(2)]
            for tt in range(NT if "nox" not in build_program.stages else 1):
                xi = xin[tt % 2]
                k.dma("sp", xi, d_x[tt * 128:(tt + 1) * 128, :])
                q, r = divmod(tt, 4)
                for half in range(2):
                    pb = PS.next()
                    for cc in range(4):
                        c = half * 4 + cc
                        k.tr(pb[:, cc * 128:(cc + 1) * 128], xi[:, c * 128:(c + 1) * 128], ident)
                    for cc in range(4):
                        c = half * 4 + cc
                        k.copy("act" if cc % 2 else "dve", xT[c][q][:, r * 128:(r + 1) * 128],
                               pb[:, cc * 128:(cc + 1) * 128])
            posi = A.alloc([NT], I32, "posi")
            k.dma("sp", posi, d_pos)
            posf = A.alloc([NT], F32, "posf")
            k.copy("dve", posf, posi)
            invf = A.alloc([32], F32, "invf")
            k.dma("sp", invf, d_invf)
            ang = A.alloc([NT, 32], F32, "ang")
            if "noang" not in build_program.stages:
                k.tt("dve", ang, posf.bc(2, [128, NT, 32]), invf.bc(1, [128, NT, 32]), ALU.mult)
            C1 = 6.28125
            C2 = 2.0 * math.pi - C1
            TWO_PI = 2.0 * math.pi

            def reduce_sin(dst, src_ang, shift):
                a = A.alloc([NT, 32], F32, "rs_a")
                if shift != 0.0:
                    k.ts("dve", a, src_ang, shift, ALU.add)
                else:
                    k.copy("dve", a, src_ang)
                nf = A.alloc([NT, 32], F32, "rs_n")
                k.ts("dve", nf, a, 1.0 / TWO_PI, ALU.mult)
                ni = A.alloc([NT, 32], I32, "rs_ni")
                k.copy("dve", ni, nf)
                k.copy("dve", nf, ni)
                r = A.alloc([NT, 32], F32, "rs_r")
                k.stt(r, nf, -C1, a, ALU.mult, ALU.add)
                k.stt(r, nf, -C2, r, ALU.mult, ALU.add)
                m1 = A.alloc([NT, 32], F32, "rs_m")
                k.ts("dve", m1, r, math.pi, ALU.is_gt)
                k.stt(r, m1, -TWO_PI, r, ALU.mult, ALU.add)
                k.ts("dve", m1, r, -math.pi, ALU.is_lt)
                k.stt(r, m1, TWO_PI, r, ALU.mult, ALU.add)
                k.ts("dve", r, r, math.pi, ALU.min, -math.pi, ALU.max)
                k.act(dst, r, AF.Sin)

            if "norope" not in build_program.stages:
                reduce_sin(sinT, ang, 0.0)
                reduce_sin(cosT, ang, math.pi / 2.0)
            S.barrier()
            A.release(m0)

        def norm_alloc():
            return dict(sq=[A.alloc([512], F32, "sq%d" % i) for i in range(2)],
                        sd=A.alloc([512], F32, "sd"), rstd=A.alloc([512], F32, "rstd"),
                        tmp=[A.alloc([512], F32, "ntmp%d" % i) for i in range(2)])

        def norm_quad(q, scale_cols, shift_cols, out_bf, out_f32=None, nt=None):
            sq, sd, rstd, tmp = nt["sq"], nt["sd"], nt["rstd"], nt["tmp"]
            pss = PS.next()
            for c in range(KC):
                k.act(sq[c % 2], xT[c][q], AF.Square)
                k.mm(pss, ones, sq[c % 2], start=(c == 0), stop=(c == KC - 1))
            k.act(sd, pss, AF.Sqrt, scale=1.0 / D, bias=EPS)
            k.recip(rstd, sd)
            for c in range(KC):
                t = tmp[c % 2]
                k.tt("dve" if c % 2 else "pool", t, xT[c][q], rstd, ALU.mult)
                if out_f32 is not None:
                    k.act(out_f32[:, c, :], t, AF.Identity, scale=scale_cols[c], bias=shift_cols[c])
                    k.copy("pool", out_bf[:, c, :], out_f32[:, c, :])
                else:
                    k.act(out_bf[:, c, :], t, AF.Identity, scale=scale_cols[c], bias=shift_cols[c])

        def norm_all(scale_cols, shift_cols):
            mN = A.mark()
            nt = norm_alloc()
            for q in range(NQ):
                norm_quad(q, scale_cols, shift_cols, hT[q], nt=nt)
            S.barrier()
            A.release(mN)

        def mod_cols(l, j):
            return [mod[:, l, j * 8 + c: j * 8 + c + 1] for c in range(KC)]

        def rope(dst, src, tt, tmp4):
            sv = src.re("p (g two f) -> p g two f", g=8, two=2)
            dv = dst.re("p (g two f) -> p g two f", g=8, two=2)
            x1, x2 = sv[:, :, 0, :], sv[:, :, 1, :]
            cs = cosT[:, tt, :].bc(1, [128, 8, 32])
            sn = sinT[:, tt, :].bc(1, [128, 8, 32])
            t1, t2, t3, t4 = [t.re("p (g f) -> p g f", g=8) for t in tmp4]
            k.tt("dve", t1, x1, cs, ALU.mult)
            k.tt("pool", t2, x2, sn, ALU.mult)
            k.tt("dve", dv[:, :, 0, :], t1, t2, ALU.subtract)
            k.tt("pool", t3, x2, cs, ALU.mult)
            k.tt("dve", t4, x1, sn, ALU.mult)
            k.tt("pool", dv[:, :, 1, :], t3, t4, ALU.add)

        def linattn_core(tt, q_dec, k_dec, k2, v_sb, dec, Sf, Sb, qkT_sb, sT_sb):
            QT = PS.next()
            QTb = QT.bitcast(BF16).re("p (g f) -> p g f", g=8)
            for h in range(4):
                k.tr(QTb[0:64, h, :], q_dec[:, h * 64:(h + 1) * 64], identb)
                k.tr(QTb[0:64, 4 + h, :], k_dec[:, h * 64:(h + 1) * 64], identb)
            k.copy("act", qkT_sb[0:64], QTb[0:64])
            ST = PS.next()
            for h in range(4):
                k.mm(ST[:, h * 128:(h + 1) * 128], qkT_sb[0:64, 4 + h, :], qkT_sb[0:64, h, :])
            k.tt("dve", sT_sb, ST.re("p (h f) -> p h f", h=4), tri.bc(1, [128, 4, 128]), ALU.mult)
            O = PS.next()
            for h in range(4):
                k.mm(O[:, h * 128:(h + 1) * 128], sT_sb[:, h, :], v_sb[:, h * 128:(h + 1) * 128],
                     start=True, stop=(tt == 0))
                if tt > 0:
                    k.mm(O[:, h * 128:(h + 1) * 128], qkT_sb[0:64, h, :], Sb[0:64, h, :],
                         start=False, stop=True)
            if tt < NT - 1:
                KV = PS.next()
                for h in range(4):
                    k.mm(KV[0:64, h * 128:(h + 1) * 128], k2[:, h * 64:(h + 1) * 64],
                         v_sb[:, h * 128:(h + 1) * 128])
                for h in range(4):
                    if tt == 0:
                        k.copy("dve", Sf[0:64, h, :], KV[0:64, h * 128:(h + 1) * 128])
                    else:
                        k.stt(Sf[0:64, h, :], Sf[0:64, h, :], dec[0:64, h:h + 1],
                              KV[0:64, h * 128:(h + 1) * 128], ALU.mult, ALU.add)
                k.copy("act", Sb[0:64], Sf[0:64])
            return O

        def head_rms(O, o_sb, osq, ssq, rs):
            k.copy("act", o_sb, O)
            k.tt("pool", osq, o_sb, o_sb, ALU.mult)
            k.red("dve", ssq, osq.re("p (h f) -> p h f", h=4), ALU.add)
            k.ts("dve", ssq, ssq, 1.0 / 128.0, ALU.mult, EPS, ALU.add)
            k.tt("pool", rs, ssq, mhalf[:, 0:4], ALU.pow)

        def out_proj(tt, o_tok, nk, wo_sb, kc0, gate_cols, oT_sb, src_bufs=None):
            q, r = divmod(tt, 4)
            OT = PS.next()
            OTb = OT.bitcast(BF16)
            for kc in range(nk):
                src = o_tok[:, kc * 128:(kc + 1) * 128]
                if src_bufs is not None:
                    src = V(src.ap, src_bufs[kc].buf)
                k.tr(OTb[:, kc * 128:(kc + 1) * 128], src, identb)
            k.copy("act", oT_sb[:, 0:nk, :], OTb[:, 0:nk * 128].re("p (g f) -> p g f", g=nk))
            for half in range(2):
                Y = PS.next()
                for dd in range(4):
                    dc = half * 4 + dd
                    for kc in range(nk):
                        k.mm(Y[:, dd * 128:(dd + 1) * 128], wo_sb[:, kc0 + kc, dc * 128:(dc + 1) * 128],
                             oT_sb[:, kc, :], start=(kc == 0), stop=(kc == nk - 1))
                for dd in range(4):
                    dc = half * 4 + dd
                    xs = xT[dc][q][:, r * 128:(r + 1) * 128]
                    k.stt(xs, Y[:, dd * 128:(dd + 1) * 128], gate_cols[dc], xs, ALU.mult, ALU.add)

        def load_bc_row(dst, dsrc, n):
            k.dma("sp", dst, V(dsrc.ap.partition_broadcast(128).rearrange("p o n -> p (o n)"), dsrc.buf))

        def even_mixer(l):
            gate1 = mod_cols(l, 2)
            mE = A.mark()
            PS.set_rot(range(8))
            norm_all(mod_cols(l, 1), mod_cols(l, 0))
            wdv = d_ewin.re("(kc p) n -> p kc n", p=128)
            mB = A.mark()
            NTB = 0 if "nogla" in build_program.stages else NT
            wa_ = A.alloc([KC, 1552], BF16, "wga")
            for (c0, c1) in ((0, 512), (512, 1024), (1024, 1552)):
                k.dma("pool", wa_[:, :, c0:c1], wdv[:, :, c0:c1])
            wo = A.alloc([4, D], BF16, "wo")
            k.dma("pool", wo, d_ewout.re("(kc p) n -> p kc n", p=128)[:, 0:4, :])
            wg = A.alloc([256], F32, "wg")
            k.dma("sp", wg[0:16, :], d_wg)
            bg = A.alloc([256], F32, "bg")
            k.dma("sp", bg[0:1, :], d_bg)
            ggb = A.alloc([128], F32, "ggb")
            load_bc_row(ggb, d_glag, 128)
            Sf = A.alloc([4, 128], F32, "Sf")
            Sb = A.alloc([4, 128], BF16, "Sb")
            grT = A.alloc([128], F32, "grT")
            ax = A.alloc([256], F32, "ax")
            ln_ = A.alloc([256], F32, "ln")
            la = A.alloc([256], F32, "la")
            b_sb = A.alloc([256], F32, "b_sb")
            eb = A.alloc([256], F32, "eb")
            enb = A.alloc([256], F32, "enb")
            e2 = A.alloc([256], F32, "e2")
            dec = A.alloc([4], F32, "dec")
            q_dec = A.alloc([256], BF16, "q_dec")
            k_dec = A.alloc([256], BF16, "k_dec")
            k2 = A.alloc([256], BF16, "k2")
            v_sb = A.alloc([512], BF16, "v_sb")
            qkT_sb = A.alloc([8, 128], BF16, "qkT_sb")
            sT_sb = A.alloc([4, 128], BF16, "sT_sb")
            o_sb = A.alloc([512], F32, "o_sb")
            osq = A.alloc([512], F32, "osq")
            ssq = A.alloc([4], F32, "ssq")
            rs = A.alloc([4], F32, "rs")
            sg = A.alloc([512], F32, "sg")
            o_tok = A.alloc([512], BF16, "o_tok")
            oT_sb = A.alloc([8, 128], BF16, "oT_sb")
            hand = [dict(q_dec=A.alloc([256], BF16, "q_dec%d" % i), k_dec=A.alloc([256], BF16, "k_dec%d" % i),
                         k2=A.alloc([256], BF16, "k2%d" % i), v_sb=A.alloc([512], BF16, "v_sb%d" % i),
                         dec=A.alloc([4], F32, "dec%d" % i), sg=A.alloc([512], F32, "sg%d" % i)) for i in range(2)]

            def gla_A(tt):
                hd = hand[tt % 2]
                q_dec, k_dec, k2, v_sb, dec, sg = hd["q_dec"], hd["k_dec"], hd["k2"], hd["v_sb"], hd["dec"], hd["sg"]
                h_t = hT_tile(tt)
                ZA = PS.fixed(0)
                ZB, ZG, ZR = PS.next(), PS.next(), PS.next()
                for (Zp, c0) in ((ZA, 0), (ZB, 512), (ZG, 1040)):
                    for kc in range(KC):
                        k.mm(Zp, h_t[:, kc, :], wa_[:, kc, c0:c0 + 512], start=(kc == 0), stop=(kc == KC - 1))
                for kc in range(KC):
                    k.mm(ZR[0:16, 0:128], wa_[:, kc, 1024:1040], h_t[:, kc, :],
                         start=(kc == 0), stop=(kc == KC - 1))
                k.copy("act", grT[0:16, :], ZR[0:16, 0:128])
                k.act(sg, ZG, AF.Silu)
                k.copy("act", v_sb, ZB)
                GP = PS.next()
                k.mm(GP[:, 0:256], grT[0:16, :], wg[0:16, :], start=True, stop=False)
                k.mm(GP[:, 0:256], ones[0:1, :], bg[0:1, :], start=False, stop=True)
                k.act(ax, GP[:, 0:256], AF.Abs)
                k.act(ax, ax, AF.Exp, scale=-1.0)
                k.act(ln_, ax, AF.Ln, bias=1.0)
                k.ts("dve", la, GP[:, 0:256], 0.0, ALU.min)
                k.tt("dve", la, la, ln_, ALU.subtract)
                B2 = PS.next()
                k.mm(B2[:, 0:256], tri16, la)
                k.mm(B2[:, 256:512], ones16, la)
                DC = PS.next()
                for h in range(4):
                    k.mm(DC[0:64, h:h + 1], la[:, h * 64:(h + 1) * 64], ones16[:, 0:1])
                k.copy("dve", b_sb, B2[:, 0:256])
                k.act(eb, B2[:, 0:256], AF.Exp)
                k.act(enb, B2[:, 0:256], AF.Exp, scale=-1.0)
                k.tt("dve", e2, B2[:, 256:512], b_sb, ALU.subtract)
                k.act(e2, e2, AF.Exp)
                k.act(dec[0:64, :], DC[0:64, 0:4], AF.Exp)
                k.stt(q_dec, ZA[:, 0:256], 0.125, eb, ALU.mult, ALU.mult)
                k.tt("dve", k_dec, ZA[:, 256:512], enb, ALU.mult)
                k.tt("dve", k2, ZA[:, 256:512], e2, ALU.mult)
                k.tt("pool", sg.re("p (h f) -> p h f", h=4), sg.re("p (h f) -> p h f", h=4),
                     ggb.bc(1, [128, 4, 128]), ALU.mult)

            def gla_B(tt):
                hd = hand[tt % 2]
                q_dec, k_dec, k2, v_sb, dec, sg = hd["q_dec"], hd["k_dec"], hd["k2"], hd["v_sb"], hd["dec"], hd["sg"]
                O = linattn_core(tt, q_dec, k_dec, k2, v_sb, dec, Sf, Sb, qkT_sb, sT_sb)
                head_rms(O, o_sb, osq, ssq, rs)
                if d_dbg is not None and tt == build_program.dbg_tile:
                    dump(la, 256); dump(eb, 256); dump(q_dec, 256); dump(k_dec, 256)
                    dump(sT_sb.re("p h f -> p (h f)"), 512); dump(o_sb, 512); dump(v_sb, 512)
                k.tt("dve", o_sb.re("p (h f) -> p h f", h=4), o_sb.re("p (h f) -> p h f", h=4),
                     rs.bc(2, [128, 4, 128]), ALU.mult)
                k.tt("pool", o_tok, o_sb, sg, ALU.mult)
                if d_dbg is not None and tt == build_program.dbg_tile:
                    dump(o_tok, 512)
                out_proj(tt, o_tok, 4, wo, 0, gate1, oT_sb)

            if NTB:
                interleave(lambda: gla_A(0), [1, 2, 3], None, [4, 5, 6, 7])
            for tt in range(NTB):
                interleave((lambda t=tt: gla_A(t + 1)) if tt + 1 < NTB else None, [1, 2, 3],
                           lambda t=tt: gla_B(t), [4, 5, 6, 7])
            S.barrier()
            A.release(mB)
            qT = A.alloc([4, L], BF16, "qT")
            kT = A.alloc([4, L], BF16, "kT")
            vA = A.alloc([NT, 4, 129], BF16, "vA")
            qmax = A.alloc([8], F32, "qmax")
            kmax = A.alloc([8], F32, "kmax")
            k.memset("dve", qmax, 0.0)
            k.memset("dve", kmax, 0.0)
            k.memset("pool", vA[:, :, :, 128:129], 1.0)
            mA = A.mark()
            wd = A.alloc([KC, 1536], BF16, "wd")
            wdv = d_ewin.re("(kc p) n -> p kc n", p=128)
            for g in range(3):
                k.dma("pool", wd[:, :, g * 512:(g + 1) * 512], wdv[:, :, 1552 + g * 512:1552 + (g + 1) * 512])
            q_sb = A.alloc([512], F32, "q_sb")
            k_sb = A.alloc([512], F32, "k_sb")
            q_rot = A.alloc([512], BF16, "q_rot")
            k_rot = A.alloc([512], BF16, "k_rot")
            rt = [A.alloc([256], F32, "rt%d" % i) for i in range(4)]
            sqt = A.alloc([512], F32, "sqt")
            nrm = A.alloc([8], F32, "nrm")
            for tt in range(NT):
                h_t = hT_tile(tt)
                Z = [PS.next() for _ in range(3)]
                for g in range(3):
                    for kc in range(KC):
                        k.mm(Z[g], h_t[:, kc, :], wd[:, kc, g * 512:(g + 1) * 512],
                             start=(kc == 0), stop=(kc == KC - 1))
                k.copy("act", vA[:, tt, :, 0:128], Z[2].re("p (h f) -> p h f", h=4))
                for (zz, sb, rot, dstT, mx) in ((Z[0], q_sb, q_rot, qT, qmax), (Z[1], k_sb, k_rot, kT, kmax)):
                    k.copy("act", sb, zz)
                    k.tt("pool", sqt, sb, sb, ALU.mult)
                    k.red("dve", nrm, sqt.re("p (g f) -> p g f", g=8), ALU.add)
                    k.tt("dve", mx, mx, nrm, ALU.max)
                    rope(rot, sb, tt, rt)
                    TP = PS.next()
                    TPb = TP.bitcast(BF16)
                    for h in range(4):
                        k.tr(TPb[:, h * 128:(h + 1) * 128], rot[:, h * 128:(h + 1) * 128], identb)
                    k.copy("act", dstT[:, :, tt * 128:(tt + 1) * 128],
                           TPb[:, 0:512].re("p (h f) -> p h f", h=4))
            S.barrier()
            A.release(mA)
            wo2 = A.alloc([4, D], BF16, "wo2")
            k.dma("pool", wo2, d_ewout.re("(kc p) n -> p kc n", p=128)[:, 4:8, :])
            dgb = A.alloc([128], F32, "dgb")
            load_bc_row(dgb, d_diffg, 128)
            lam_init = 0.8 - 0.6 * math.exp(-0.3 * l)
            k.ts("dve", dgb, dgb, 1.0 - lam_init, ALU.mult)
            lin = A.alloc([256], F32, "lin")
            k.dma("sp", lin[0:1, :], d_lam)
            lp = A.alloc([128], F32, "lp")
            k.tt("dve", lp[0:1, :], lin[0:1, 0:128], lin[0:1, 128:256], ALU.mult)
            ls = A.alloc([2], F32, "ls")
            k.red("dve", ls[0:1, :], lp[0:1, :].re("p (g f) -> p g f", g=2), ALU.add)
            k.act(ls[0:1, :], ls[0:1, :], AF.Exp)
            nl = A.alloc([1], F32, "nl")
            k.tt("dve", nl[0:1, :], ls[0:1, 1:2], ls[0:1, 0:1], ALU.subtract)
            k.ts("dve", nl[0:1, :], nl[0:1, :], -lam_init, ALU.add)
            PL = PS.next()
            k.mm(PL[:, 0:1], ones[0:1, :], nl[0:1, 0:1])
            neglam = A.alloc([1], F32, "neglam")
            k.copy("dve", neglam, PL[:, 0:1])
            PQ = PS.next()
            k.tr(PQ[0:8, 0:128], qmax, ident)
            k.tr(PQ[0:8, 128:256], kmax, ident)
            m2 = A.alloc([2], F32, "m2")
            k.red("dve", m2[0:8, :], PQ[0:8, 0:256].re("p (g f) -> p g f", g=2), ALU.max)
            mm_ = A.alloc([1], F32, "mm_")
            k.tt("dve", mm_[0:8, :], m2[0:8, 0:1], m2[0:8, 1:2], ALU.mult)
            k.act(mm_[0:8, :], mm_[0:8, :], AF.Sqrt)
            dg = A.alloc([8], F32, "dg")
            k.ts("dve", dg[0:8, :], ident[0:8, 0:8], mm_[0:8, 0:1], ALU.mult, -0.125, ALU.mult)
            PM = PS.next()
            k.mm(PM[:, 0:8], ones[0:8, :], dg[0:8, :])
            negM = A.alloc([8], F32, "negM")
            k.copy("dve", negM, PM[:, 0:8])
            res = [dict(PT=[A.alloc([512], BF16, "PT%d_%d" % (s_, i)) for i in range(2)],
                        oa=A.alloc([128], F32, "oa%d" % s_), od=A.alloc([128], F32, "od%d" % s_),
                        r01=A.alloc([2], F32, "r01%d" % s_), dsq=A.alloc([128], F32, "dsq%d" % s_),
                        dss=A.alloc([1], F32, "dss%d" % s_), npt=0, pend=[],
                        acc=[PS.fixed(2 * s_), PS.fixed(2 * s_ + 1)]) for s_ in range(2)]
            o_tok2 = A.alloc([512], BF16, "o_tok2")
            o_tok2_h = [V(o_tok2.ap[:, h * 128:(h + 1) * 128], Buf("o_tok2_%d" % h)) for h in range(4)]
            oT2 = A.alloc([8, 128], BF16, "oT2")
            PS.set_rot([4, 5, 6, 7])

            def head_block(i, h, R):
                PT, oa, od, r01, dsq, dss, acc, pend = (R["PT"], R["oa"], R["od"], R["r01"], R["dsq"],
                                                        R["dss"], R["acc"], R["pend"])

                def flush_pv():
                    while pend:
                        pend.pop(0)()
                for m in range(2):
                    hm = h * 2 + m
                    for jb in range(0, i + 1, 4):
                        nj = min(4, i + 1 - jb)
                        SB = PS.next()
                        for jj in range(nj):
                            j = jb + jj
                            k.mm(SB[:, jj * 128:(jj + 1) * 128],
                                 kT[m * 64:(m + 1) * 64, h, j * 128:(j + 1) * 128],
                                 qT[m * 64:(m + 1) * 64, h, i * 128:(i + 1) * 128])
                        flush_pv()
                        P = PT[R["npt"] % 2]
                        R["npt"] += 1
                        k.act(P[:, 0:nj * 128], SB[:, 0:nj * 128], AF.Exp, scale=0.125,
                              bias=negM[:, hm:hm + 1])
                        if jb + nj == i + 1:
                            dsl = P[:, (nj - 1) * 128:nj * 128]
                            k.tt("pool", dsl, dsl, trib, ALU.mult)

                        def pv(P=P, nj=nj, jb=jb, m=m):
                            for jj in range(nj):
                                j = jb + jj
                                k.mm(acc[m][:, 0:129], P[:, jj * 128:(jj + 1) * 128], vA[:, j, h, :],
                                     start=(j == 0), stop=(j == i))
                        pend.append(pv)
                flush_pv()
                k.recip(r01[:, 0:1], acc[0][:, 128:129])
                k.recip(r01[:, 1:2], acc[1][:, 128:129])
                k.tt("dve", r01[:, 1:2], r01[:, 1:2], neglam, ALU.mult)
                k.ts("dve", oa, acc[0][:, 0:128], r01[:, 0:1], ALU.mult)
                k.stt(od, acc[1][:, 0:128], r01[:, 1:2], oa, ALU.mult, ALU.add)
                k.tt("pool", dsq, od, od, ALU.mult)
                k.red("dve", dss, dsq, ALU.add)
                k.ts("dve", dss, dss, 1.0 / 128.0, ALU.mult, EPS, ALU.add)
                k.tt("pool", dss, dss, mhalf[:, 0:1], ALU.pow)
                k.stt(o_tok2_h[h], od, dss[:, 0:1], dgb, ALU.mult, ALU.mult)

            o_tok2_all = V(o_tok2.ap, None)
            for i in range(0 if "nodiff" in build_program.stages else NT):
                interleave(lambda i=i: (head_block(i, 0, res[0]), head_block(i, 2, res[0])), [4, 5],
                           lambda i=i: (head_block(i, 1, res[1]), head_block(i, 3, res[1])), [6, 7])
                out_proj(i, o_tok2, 4, wo2, 0, gate1, oT2, src_bufs=o_tok2_h)
            S.barrier()
            PS.set_rot(range(8))
            A.release(mE)

        def odd_mixer(l):
            gate1 = mod_cols(l, 2)
            mO = A.mark()
            PS.set_rot(range(8))
            norm_all(mod_cols(l, 1), mod_cols(l, 0))
            wi = A.alloc([KC, 2560], BF16, "wi")
            wv = d_owin.re("(kc p) n -> p kc n", p=128)
            for g in range(5):
                k.dma("pool", wi[:, :, g * 512:(g + 1) * 512], wv[:, :, g * 512:(g + 1) * 512])
            wo = A.alloc([KC, D], BF16, "wo")
            k.dma("pool", wo, d_owout.re("(kc p) n -> p kc n", p=128))
            lngb = A.alloc([512], F32, "lngb")
            lnbb = A.alloc([512], F32, "lnbb")
            load_bc_row(lngb, d_lng, 512)
            load_bc_row(lnbb, d_lnb, 512)
            wsb = A.alloc([4, 128], BF16, "wsb")
            mW = A.mark()
            wsf = A.alloc([4, 128], F32, "wsf")
            k.dma("sp", wsf, d_swT.re("g j i -> j g i"))
            k.tt("dve", wsb, wsf, tri.bc(1, [128, 4, 128]), ALU.mult)
            S.barrier()
            A.release(mW)
            bsT = A.alloc([4], F32, "bsT")
            k.dma("sp", bsT, d_sbT)
            p1 = A.alloc([1], F32, "p1")
            k.ts("dve", p1, iof[:, 0:1], -1.0, ALU.mult, 1.0, ALU.add)
            EB = A.alloc([4, 64], F32, "EB")
            ENB = A.alloc([4, 64], F32, "ENB")
            E2 = A.alloc([4, 64], F32, "E2")
            dec = A.alloc([4], F32, "dec")
            tmpc = A.alloc([64], F32, "tmpc")
            for h in range(4):
                lg = math.log(1.0 - 2.0 ** (-5.0 - h))
                k.ts("dve", tmpc, ones[:, 0:64], p1[:, 0:1], ALU.mult)
                k.act(EB[:, h, :], tmpc, AF.Exp, scale=lg)
                k.ts("dve", EB[:, h, :], EB[:, h, :], 0.125, ALU.mult)
                k.act(ENB[:, h, :], tmpc, AF.Exp, scale=-lg)
                k.ts("dve", tmpc, tmpc, -1.0, ALU.mult, 128.0, ALU.add)
                k.act(E2[:, h, :], tmpc, AF.Exp, scale=lg)
                k.memset("dve", dec[:, h:h + 1], math.exp(lg * 128.0))
            EBv, ENBv, E2v = [t.re("p h f -> p (h f)") for t in (EB, ENB, E2)]
            Sf = A.alloc([4, 128], F32, "Sf")
            Sb = A.alloc([4, 128], BF16, "Sb")
            u = A.alloc([512], F32, "u")
            gv = A.alloc([512], F32, "gv")
            st6 = A.alloc([6], F32, "st6")
            mv = A.alloc([2], F32, "mv")
            rs1 = A.alloc([1], F32, "rs1")
            nmr = A.alloc([1], F32, "nmr")
            svn = A.alloc([512], BF16, "svn")
            qk_sb = A.alloc([512], F32, "qk_sb")
            qk_rot = A.alloc([512], F32, "qk_rot")
            rt2 = [A.alloc([256], F32, "rt%d" % i) for i in range(2)]
            rt = [rt2[0], rt2[1], rt2[0], rt2[1]]
            qkT_sb = A.alloc([8, 128], BF16, "qkT_sb")
            sT_sb = A.alloc([4, 128], BF16, "sT_sb")
            o_sb = A.alloc([512], F32, "o_sb")
            osq = A.alloc([512], F32, "osq")
            ssq = A.alloc([4], F32, "ssq")
            rs = A.alloc([4], F32, "rs")
            oT_sb = A.alloc([8, 128], BF16, "oT_sb")
            hand = [dict(q_dec=A.alloc([256], BF16, "q_dec%d" % i), k_dec=A.alloc([256], BF16, "k_dec%d" % i),
                         k2=A.alloc([256], BF16, "k2%d" % i), v_sb=A.alloc([512], BF16, "v_sb%d" % i),
                         sg=A.alloc([512], F32, "sg%d" % i), o_tok=A.alloc([1024], BF16, "o_tok%d" % i))
                    for i in range(2)]

            def odd_A(tt):
                hd = hand[tt % 2]
                q_dec, k_dec, k2, v_sb, sg, o_tok = (hd["q_dec"], hd["k_dec"], hd["k2"], hd["v_sb"],
                                                     hd["sg"], hd["o_tok"])
                h_t = hT_tile(tt)
                Z = []
                for g in range(4):
                    Z.append(PS.next())
                    for kc in range(KC):
                        k.mm(Z[g], h_t[:, kc, :], wi[:, kc, g * 512:(g + 1) * 512],
                             start=(kc == 0), stop=(kc == KC - 1))
                k.act(u, Z[0], AF.Gelu)
                k.act(gv, Z[1], AF.Gelu)
                k.copy("act", qk_sb, Z[2])
                k.copy("act", v_sb, Z[3])
                Z4 = PS.next()
                for kc in range(KC):
                    k.mm(Z4, h_t[:, kc, :], wi[:, kc, 2048:2560], start=(kc == 0), stop=(kc == KC - 1))
                k.act(sg, Z4, AF.Silu)
                S.op("dve", lambda e: e.bn_stats(out=st6.ap, in_=gv.ap), reads=[gv.buf], writes=[st6.buf])
                S.op("dve", lambda e: e.bn_aggr(out=mv.ap, in_=st6.ap), reads=[st6.buf], writes=[mv.buf])
                k.ts("dve", rs1, mv[:, 1:2], EPS, ALU.add)
                k.tt("pool", rs1, rs1, mhalf[:, 0:1], ALU.pow)
                k.stt(nmr, mv[:, 0:1], -1.0, rs1, ALU.mult, ALU.mult)
                k.act(gv, gv, AF.Identity, scale=rs1[:, 0:1], bias=nmr[:, 0:1])
                k.tt("pool", gv, gv, lngb, ALU.mult)
                k.tt("pool", svn, gv, lnbb, ALU.add)
                SG = PS.next()
                for g in range(4):
                    k.mm(SG[:, g * 128:(g + 1) * 128], wsb[:, g, :], svn[:, g * 128:(g + 1) * 128])
                for g in range(4):
                    k.stt(o_tok[:, g * 128:(g + 1) * 128], SG[:, g * 128:(g + 1) * 128], bsT[:, g:g + 1],
                          u[:, g * 128:(g + 1) * 128], ALU.add, ALU.mult)
                rope(qk_rot, qk_sb, tt, rt)
                k.tt("dve", q_dec, qk_rot[:, 0:256], EBv, ALU.mult)
                k.tt("pool", k_dec, qk_rot[:, 256:512], ENBv, ALU.mult)
                k.tt("dve", k2, qk_rot[:, 256:512], E2v, ALU.mult)

            def odd_B(tt):
                hd = hand[tt % 2]
                q_dec, k_dec, k2, v_sb, sg, o_tok = (hd["q_dec"], hd["k_dec"], hd["k2"], hd["v_sb"],
                                                     hd["sg"], hd["o_tok"])
                O = linattn_core(tt, q_dec, k_dec, k2, v_sb, dec, Sf, Sb, qkT_sb, sT_sb)
                head_rms(O, o_sb, osq, ssq, rs)
                k.tt("dve", o_sb.re("p (h f) -> p h f", h=4), o_sb.re("p (h f) -> p h f", h=4),
                     rs.bc(2, [128, 4, 128]), ALU.mult)
                k.tt("pool", o_tok[:, 512:1024], o_sb, sg, ALU.mult)
                out_proj(tt, o_tok, 8, wo, 0, gate1, oT_sb)

            interleave(lambda: odd_A(0), [0, 1, 2, 3], None, [4, 5, 6, 7])
            for tt in range(NT):
                interleave((lambda t=tt: odd_A(t + 1)) if tt + 1 < NT else None, [0, 1, 2, 3],
                           lambda t=tt: odd_B(t), [4, 5, 6, 7])
            S.barrier()
            A.release(mO)

        def moe(l):
            gate2 = mod_cols(l, 5)
            mM = A.mark()
            PS.set_rot(range(8))
            GT = A.alloc([L], F32, "GT")
            bo = A.alloc([D], F32, "bo")
            k.dma("sp", bo[0:NE, :], d_ebo[l])
            biT = A.alloc([NE, 16], F32, "biT")
            k.dma("sp", biT, d_ebi[l])
            bi1 = A.alloc([NE, 8], F32, "bi1")
            k.ts("dve", bi1, biT[:, :, 8:16], 1.0, ALU.add)
            ring = [[A.alloc([KC, 512], BF16, "wr%d_%d" % (s, i)) for i in range(2)] +
                    [A.alloc([4, D], BF16, "wr%d_2" % s)] for s in range(2)]
            wiv = [d_ewi[l, e].re("(kc p) n -> p kc n", p=128) for e in range(NE)]
            wov = [d_ewo[l, e].re("(kc p) n -> p kc n", p=128) for e in range(NE)]

            def load_half(he):
                e, hh = divmod(he, 2)
                s = he % 2
                k.dma("pool", ring[s][0], wiv[e][:, :, hh * 512:(hh + 1) * 512])
                k.dma("pool", ring[s][1], wiv[e][:, :, D + hh * 512:D + (hh + 1) * 512])
                k.dma("pool", ring[s][2], wov[e][:, hh * 4:(hh + 1) * 4, :])

            load_half(0)
            load_half(1)
            m1 = A.mark()
            rw = A.alloc([KC, NE], F32, "rw")
            k.dma("sp", rw, d_rw[l].re("(kc p) e -> p kc e", p=128))
            rb = A.alloc([NE], F32, "rb")
            k.dma("sp", rb[0:1, :], d_rb[l])
            hf = A.alloc([KC, 512], F32, "hf")
            lg4 = [A.alloc([4, NE], F32, "lg4_%d" % i) for i in range(2)]
            t84 = [A.alloc([4, 8], F32, "t84_%d" % i) for i in range(2)]
            ex4 = [A.alloc([4, NE], F32, "ex4_%d" % i) for i in range(2)]
            msk4 = [A.alloc([4, NE], F32, "msk4_%d" % i) for i in range(2)]
            sm4 = [A.alloc([4], F32, "sm4_%d" % i) for i in range(2)]
            ntm = norm_alloc()

            def router_mm(q):
                LG = PS.next()
                for r in range(4):
                    for kc in range(KC):
                        k.mm(LG[:, r * NE:(r + 1) * NE], hf[:, kc, r * 128:(r + 1) * 128], rw[:, kc, :],
                             start=(kc == 0), stop=False)
                    k.mm(LG[:, r * NE:(r + 1) * NE], ones[0:1, :], rb[0:1, :], start=False, stop=True)
                k.copy("act", lg4[q % 2], LG[:, 0:4 * NE].re("p (r e) -> p r e", r=4))

            def route_elem(q):
                lg, t8, ex, msk, sm = lg4[q % 2], t84[q % 2], ex4[q % 2], msk4[q % 2], sm4[q % 2]
                for r in range(4):
                    S.op("dve", lambda e, r=r: e.max(out=t8.ap[:, r, :], in_=lg.ap[:, r, :]),
                         reads=[lg.buf], writes=[t8.buf])
                k.tt("dve", ex, lg, V(t8.ap[:, :, 0:1].broadcast_to([128, 4, NE]), t8.buf), ALU.subtract)
                k.act(ex, ex, AF.Exp)
                k.tt("dve", msk, lg, V(t8.ap[:, :, 3:4].broadcast_to([128, 4, NE]), t8.buf), ALU.is_ge)
                k.tt("dve", ex, ex, msk, ALU.mult)
                k.red("dve", sm, ex, ALU.add)
                k.recip(sm, sm)
                k.tt("dve", ex, ex, sm.bc(2, [128, 4, NE]), ALU.mult)
                TG = PS.next()
                for r in range(4):
                    k.tr(TG[0:NE, r * 128:(r + 1) * 128], ex[:, r, :], ident)
                k.copy("act", GT[0:NE, q * 512:(q + 1) * 512], TG[0:NE, :])

            norm_quad(0, mod_cols(l, 4), mod_cols(l, 3), hT[0], out_f32=hf, nt=ntm)
            for q in range(NQ):
                router_mm(q)
                if q + 1 < NQ:
                    norm_quad(q + 1, mod_cols(l, 4), mod_cols(l, 3), hT[q + 1], out_f32=hf, nt=ntm)
                route_elem(q)
            GTb = A.alloc([L], BF16, "GTb")
            k.copy("dve", GTb[0:NE, :], GT[0:NE, :])
            k.dma("sp", d_gtb[l], GTb[0:NE, :])
            S.barrier()
            A.release(m1)
            for q in range(NQ):
                for dc in range(KC):
                    Y = PS.next()
                    k.mm(Y, bo[0:NE, dc * 128:(dc + 1) * 128], GT[0:NE, q * 512:(q + 1) * 512])
                    k.stt(xT[dc][q], Y, gate2[dc], xT[dc][q], ALU.mult, ALU.add)
            aT = [A.alloc([4, 512], BF16, "aT%d" % i) for i in range(2)]
            gate_bc = [A.alloc([L], BF16, "gate_bc%d" % i) for i in range(2)]

            def load_gate(e):
                src = d_gtb[l][e:e + 1, :]
                k.dma("sp", gate_bc[e % 2],
                      V(src.ap.partition_broadcast(128).rearrange("p o n -> p (o n)"), src.buf))
            gt = [A.alloc([512], F32, "g%d" % i) for i in range(2)]
            sgt = [A.alloc([512], F32, "s%d" % i) for i in range(2)]
            lt = [A.alloc([512], F32, "l%d" % i) for i in range(2)]
            ut = [A.alloc([512], F32, "u%d" % i) for i in range(2)]
            PS.set_rot([0, 1, 2, 3])
            ybank = [PS.fixed(4), PS.fixed(5), PS.fixed(6), PS.fixed(7)]
            load_gate(0)
            load_gate(1)
            state = {"cnt": 0, "ny": 0}

            def z_part(n, he, q):
                e, hh = divmod(he, 2)
                w_glu, w_lin, w_o = ring[he % 2]
                gs_ = gate_bc[e % 2][:, q * 512:(q + 1) * 512]
                a_ = aT[n % 2]
                pend = None
                for fc in range(4):
                    i3 = state["cnt"] % 2
                    state["cnt"] += 1
                    ZG_ = PS.next()
                    for kc in range(KC):
                        k.mm(ZG_, w_glu[:, kc, fc * 128:(fc + 1) * 128], hT[q][:, kc, :],
                             start=(kc == 0), stop=(kc == KC - 1))
                    ZL_ = PS.next()
                    for kc in range(KC):
                        k.mm(ZL_, w_lin[:, kc, fc * 128:(fc + 1) * 128], hT[q][:, kc, :],
                             start=(kc == 0), stop=(kc == KC - 1))
                    col = hh * 4 + fc
                    k.ts("dve", gt[i3], ZG_, biT[:, e, col:col + 1], ALU.add, 7.0, ALU.min)
                    k.act(sgt[i3], gt[i3], AF.Sigmoid, scale=1.702)
                    k.tt("pool", gt[i3], gt[i3], sgt[i3], ALU.mult)
                    k.act(lt[i3], ZL_, AF.Identity, bias=bi1[:, e, col:col + 1])
                    k.ts("dve", lt[i3], lt[i3], -6.0, ALU.max, 8.0, ALU.min)
                    if pend is not None:
                        pf, pi = pend
                        k.tt("dve", ut[pi], gt[pi], lt[pi], ALU.mult)
                        k.tt("pool", a_[:, pf, :], ut[pi], gs_, ALU.mult)
                    pend = (fc, i3)
                pf, pi = pend
                k.tt("dve", ut[pi], gt[pi], lt[pi], ALU.mult)
                k.tt("pool", a_[:, pf, :], ut[pi], gs_, ALU.mult)

            def y_part(n, he, q):
                w_glu, w_lin, w_o = ring[he % 2]
                a_ = aT[n % 2]
                for dc in range(KC):
                    Y = ybank[state["ny"] % 4]
                    state["ny"] += 1
                    for fc in range(4):
                        k.mm(Y, w_o[:, fc, dc * 128:(dc + 1) * 128], a_[:, fc, :],
                             start=(fc == 0), stop=(fc == 3))
                    k.stt(xT[dc][q], Y, gate2[dc], xT[dc][q], ALU.mult, ALU.add)

            steps = [(he, q) for he in range(2 * NE) for q in range(NQ)]
            for n, (he, q) in enumerate(steps):
                z_part(n, he, q)
                if n > 0:
                    y_part(n - 1, *steps[n - 1])
                if q == 1 and he >= 1 and he + 1 < 2 * NE:
                    load_half(he + 1)
                if q == 1 and he % 2 == 0 and he >= 2 and he // 2 + 1 < NE:
                    load_gate(he // 2 + 1)
            y_part(len(steps) - 1, *steps[-1])
            S.barrier()
            PS.set_rot(range(8))
            A.release(mM)

        def final_out():
            mF = A.mark()
            PS.set_rot(range(8))
            hf = A.alloc([KC, 512], F32, "hfin")
            dummy = A.alloc([KC, 512], BF16, "dummyb")
            zero = A.alloc([1], F32, "zero")
            k.memset("dve", zero, 0.0)
            ot = [A.alloc([D], F32, "ot%d" % i) for i in range(2)]
            gcols = [fgT[:, c:c + 1] for c in range(KC)]
            zcols = [zero[:, 0:1] for c in range(KC)]
            ntf = norm_alloc()
            for q in range(NQ):
                norm_quad(q, gcols, zcols, dummy, out_f32=hf, nt=ntf)
                for r in range(4):
                    tt = q * 4 + r
                    o = ot[tt % 2]
                    for half in range(2):
                        pb = PS.next()
                        for cc in range(4):
                            c = half * 4 + cc
                            k.tr(pb[:, cc * 128:(cc + 1) * 128], hf[:, c, r * 128:(r + 1) * 128], ident)
                        k.copy("act" if half else "dve", o[:, half * 512:(half + 1) * 512], pb)
                    k.dma("sp", d_out[tt * 128:(tt + 1) * 128, :], o)
            A.release(mF)

        def write_x_raw():
            mF = A.mark()
            PS.set_rot(range(8))
            ot = [A.alloc([D], F32, "ot%d" % i) for i in range(2)]
            for tt in range(NT if "nox" not in build_program.stages else 1):
                q, r = divmod(tt, 4)
                o = ot[tt % 2]
                for half in range(2):
                    pb = PS.next()
                    for cc in range(4):
                        c = half * 4 + cc
                        k.tr(pb[:, cc * 128:(cc + 1) * 128], xT[c][q][:, r * 128:(r + 1) * 128], ident)
                    k.copy("act" if half else "dve", o[:, half * 512:(half + 1) * 512], pb)
                k.dma("sp", d_out[tt * 128:(tt + 1) * 128, :], o)
            A.release(mF)

        prologue()
        for l in layers:
            if "mix" in stages:
                if l % 2 == 0:
                    even_mixer(l)
                else:
                    odd_mixer(l)
            if "moe" in stages:
                moe(l)
        if do_final:
            final_out()
        else:
            write_x_raw()
        S.barrier()
        nsem = S.emit()
        build_program.info = dict(nsem=nsem, nwaits=S.nwaits, nops=dict(S.nops), ndma=dict(S.ndma),
                                  peak_words=A.peak)
    return nc


build_program.stages = ("mix", "moe")
build_program.info = {}
build_program.dbg_tile = 0


def make_in_maps(inp, cores=range(N_CORES)):
    f = lambda a: np.ascontiguousarray(np.asarray(a))
    half = 32
    invf = (np.float32(10000.0) ** (-(np.arange(half, dtype=np.float32) / np.float32(half)))).astype(np.float32)
    shared = {
        "invf": f(np.broadcast_to(invf[None, :], (128, half))),
        "w_ada": f(inp["w_ada"]),
        "b_adaT": f(np.asarray(inp["b_ada"]).reshape(2, 48, 128).transpose(2, 0, 1)),
        "even_w_in": f(inp["even_w_in"][0]),
        "gla_w_gate": f(inp["gla_w_gate"][0]),
        "gla_b_gate": f(inp["gla_b_gate"]),
        "gla_norm_g": f(inp["gla_norm_g"]),
        "lam_in": f(np.concatenate([inp["diff_lam_q1"][0], inp["diff_lam_q2"][0],
                                    inp["diff_lam_k1"][0], inp["diff_lam_k2"][0]])[None, :]),
        "diff_norm_g": f(inp["diff_norm_g"]),
        "even_w_out": f(inp["even_w_out"][0]),
        "odd_w_in": f(inp["odd_w_in"][0]),
        "sgu_ln_g": f(inp["sgu_ln_g"]),
        "sgu_ln_b": f(inp["sgu_ln_b"]),
        "sgu_wT": f(np.asarray(inp["sgu_w"][0]).transpose(0, 2, 1)),
        "sgu_bT": f(np.asarray(inp["sgu_b"][0]).T),
        "odd_w_out": f(inp["odd_w_out"][0]),
        "router_w": f(inp["router_w"]),
        "router_b": f(np.asarray(inp["router_b"])[:, None, :]),
        "expert_w_in": f(inp["expert_w_in"]),
        "expert_b_inT": f(np.asarray(inp["expert_b_in"]).reshape(2, NE, 16, 128).transpose(0, 3, 1, 2)),
        "expert_w_out": f(inp["expert_w_out"]),
        "expert_b_out": f(inp["expert_b_out"]),
        "final_gT": f(np.asarray(inp["final_norm_g"]).reshape(KC, 128).T),
    }
    maps = []
    for b in cores:
        m = dict(shared)
        m["x"] = f(inp["x"][b])
        m["cT"] = f(np.asarray(inp["c"][b]).reshape(KC, 128).T)
        m["posT"] = f(np.asarray(inp["positions"][b]).astype(np.int32).reshape(NT, 128).T)
        maps.append({n: m[n] for n in build_program.in_names})
    return maps


def kernel(**inputs):
    build_program.stages = ("mix", "moe")
    nc = build_program(layers=(0, 1), do_final=True)
    maps = make_in_maps(inputs)
    res = run_bass_kernel_spmd(nc, maps, core_ids=list(range(N_CORES)))
    return np.stack([np.asarray(r["yout"]) for r in res.results], axis=0).astype(np.float32)
```
